# Optimizing a Trainium2 kernel written in Bass

```python
import math
import jax, jax.numpy as jnp
from jax import lax
import numpy as np

D_MODEL = 1024
BATCH = 8
SEQ = 4096
DEPTH = 1

PLE_DIM = 256
EPS = 1e-6
NEG_INF = -1e30

A_HEADS = 8
A_HEAD_DIM = 64
A_WIDTH = A_HEADS * A_HEAD_DIM
DILATED_BRANCHES = ((128, 1), (512, 4), (2048, 16))

B_HEADS = 8
B_NOPE = 64
B_ROPE = 32
B_VDIM = 64
B_WIDTH = B_HEADS * B_VDIM
Q_LORA = 384
KV_LORA = 256
ROPE_THETA = 10000.0
Q_BLOCK = 128

MIX_WIDTH = A_WIDTH + B_WIDTH
IN_WIDTH = 3 * A_WIDTH + Q_LORA + KV_LORA + B_ROPE

N_GROUPS = 4
EXPERTS_PER_GROUP = 8
N_EXPERTS = N_GROUPS * EXPERTS_PER_GROUP
TOP_K_INNER = 2
EXPERT_FF = 256

kernel_name = "hymba_dilated_mla_hmoe_encoder"


def rms_norm(x, g):
    xf = x.astype(jnp.float32)
    y = xf * lax.rsqrt(jnp.mean(xf * xf, axis=-1, keepdims=True) + EPS)
    return (y * g.astype(jnp.float32)).astype(x.dtype)


def alibi_slopes(n_heads):
    return jnp.exp2(-8.0 * (jnp.arange(n_heads, dtype=jnp.float32) + 1.0) / n_heads)


def rope_tables(seq, dim):
    inv_freq = 1.0 / (ROPE_THETA ** (jnp.arange(0, dim, 2, dtype=jnp.float32) / dim))
    ang = jnp.arange(seq, dtype=jnp.float32)[:, None] * inv_freq[None, :]
    return jnp.cos(ang), jnp.sin(ang)


def apply_rope(x, cos, sin):
    half = x.shape[-1] // 2
    x1 = x[..., :half].astype(jnp.float32)
    x2 = x[..., half:].astype(jnp.float32)
    return jnp.concatenate([x1 * cos - x2 * sin, x1 * sin + x2 * cos], axis=-1).astype(x.dtype)


def banded_attention(q, k, v, dist_slopes, half):
    N, L, H, Dh = q.shape
    W = half
    nb = -(-L // W)
    Lp = nb * W
    qb = jnp.pad(q, ((0, 0), (0, Lp - L), (0, 0), (0, 0))).reshape(N, nb, W, H, Dh)
    pad_kv = ((0, 0), (W, Lp - L + W), (0, 0), (0, 0))
    kp = jnp.pad(k, pad_kv)
    vp = jnp.pad(v, pad_kv)
    kb = jnp.concatenate([kp[:, s * W: s * W + Lp].reshape(N, nb, W, H, Dh) for s in range(3)], axis=2)
    vb = jnp.concatenate([vp[:, s * W: s * W + Lp].reshape(N, nb, W, H, Dh) for s in range(3)], axis=2)
    scores = jnp.einsum('nbqhd,nbkhd->nbhqk', qb, kb).astype(jnp.float32)
    qi = jnp.arange(W)[:, None]
    kc = jnp.arange(3 * W)[None, :]
    rel = kc - W - qi
    j = jnp.arange(nb)[:, None] * W + jnp.arange(3 * W)[None, :] - W
    mask = (jnp.abs(rel) <= half)[None, :, :] & ((j >= 0) & (j < L))[:, None, :]
    bias = -dist_slopes[:, None, None] * jnp.abs(rel).astype(jnp.float32)[None]
    scores = jnp.where(mask[None, :, None], scores + bias[None, None], NEG_INF)
    m = jnp.max(scores, axis=-1, keepdims=True)
    e = jnp.exp(scores - m)
    s = jnp.sum(e, axis=-1, keepdims=True)
    out = jnp.einsum('nbhqk,nbkhd->nbqhd', (e / s).astype(v.dtype), vb)
    lse = (m + jnp.log(s))[..., 0]
    out = out.reshape(N, Lp, H, Dh)[:, :L]
    lse = lse.transpose(0, 1, 3, 2).reshape(N, Lp, H)[:, :L]
    return out, lse


def dilated_mixture_attention(q, k, v):
    B, S, H, Dh = q.shape
    q = q * (Dh ** -0.5)
    slopes = alibi_slopes(H)
    outs, lses = [], []
    for window, dil in DILATED_BRANCHES:
        half = window // (2 * dil)
        L = S // dil

        def to_sub(t):
            return t.reshape(B, L, dil, H, Dh).transpose(0, 2, 1, 3, 4).reshape(B * dil, L, H, Dh)

        o, lse = banded_attention(to_sub(q), to_sub(k), to_sub(v), slopes * dil, half)
        outs.append(o.reshape(B, dil, L, H, Dh).transpose(0, 2, 1, 3, 4).reshape(B, S, H, Dh))
        lses.append(lse.reshape(B, dil, L, H).transpose(0, 2, 1, 3).reshape(B, S, H))
    w = jax.nn.softmax(jnp.stack(lses), axis=0)
    return jnp.einsum('nbsh,nbshd->bshd', w.astype(q.dtype), jnp.stack(outs))


def mla_attention(qn, qr, kn, kr, v):
    B, S, H, _ = qn.shape
    scale = (B_NOPE + B_ROPE) ** -0.5
    nq = S // Q_BLOCK
    qn_b = qn.reshape(B, nq, Q_BLOCK, H, B_NOPE).transpose(1, 0, 2, 3, 4)
    qr_b = qr.reshape(B, nq, Q_BLOCK, H, B_ROPE).transpose(1, 0, 2, 3, 4)

    def attend(args):
        qn_i, qr_i = args
        s = jnp.einsum('bqhd,bkhd->bhqk', qn_i, kn) + jnp.einsum('bqhr,bkr->bhqk', qr_i, kr)
        pr = jax.nn.softmax(s.astype(jnp.float32) * scale, axis=-1).astype(v.dtype)
        return jnp.einsum('bhqk,bkhd->bqhd', pr, v)

    out = lax.map(attend, (qn_b, qr_b))
    return out.transpose(1, 0, 2, 3, 4).reshape(B, S, H * B_VDIM)


def hierarchical_moe(t, w_r1, b_r1, w_r2, b_r2, w_e_gate, w_e_up, w_e_down):
    l1 = (t @ w_r1 + b_r1).astype(jnp.float32)
    p1 = jax.nn.softmax(l1, axis=-1)
    g_val, g_idx = lax.top_k(p1, 1)
    l2_all = jnp.einsum('td,gde->tge', t, w_r2) + b_r2
    l2 = jnp.take_along_axis(l2_all, g_idx[:, :, None], axis=1)[:, 0].astype(jnp.float32)
    p2 = jax.nn.softmax(l2, axis=-1)
    e_val, e_idx = lax.top_k(p2, TOP_K_INNER)
    gate = g_val * e_val / jnp.sum(e_val, axis=-1, keepdims=True)
    eid = g_idx * EXPERTS_PER_GROUP + e_idx
    cw = jnp.sum(jax.nn.one_hot(eid, N_EXPERTS, dtype=jnp.float32) * gate[:, :, None], axis=1)
    cw = cw.astype(t.dtype)
    y = jnp.zeros_like(t)
    for e in range(N_EXPERTS):
        h = jax.nn.silu(t @ w_e_gate[e]) * (t @ w_e_up[e])
        y = y + cw[:, e:e + 1] * (h @ w_e_down[e])
    return y


def setup_inputs(seed: int = 0) -> dict:
    key = jax.random.key(seed)
    ks = jax.random.split(key, 24)
    f32 = jnp.float32

    def nrm(k, shape, fan_in):
        return jax.random.normal(k, shape, f32) * (fan_in ** -0.5)

    def gain(k, shape):
        return 1.0 + 0.02 * jax.random.normal(k, shape, f32)

    return {
        "x": jax.random.normal(ks[0], (BATCH, SEQ, D_MODEL), f32),
        "p": jax.random.normal(ks[1], (DEPTH, BATCH, SEQ, PLE_DIM), f32),
        "g_mix": gain(ks[2], (DEPTH, D_MODEL)),
        "w_in": nrm(ks[3], (DEPTH, D_MODEL, IN_WIDTH), D_MODEL),
        "g_cq": gain(ks[4], (DEPTH, Q_LORA)),
        "w_uq": nrm(ks[5], (DEPTH, Q_LORA, B_HEADS * (B_NOPE + B_ROPE)), Q_LORA),
        "g_ckv": gain(ks[6], (DEPTH, KV_LORA)),
        "w_ukv": nrm(ks[7], (DEPTH, KV_LORA, B_HEADS * (B_NOPE + B_VDIM)), KV_LORA),
        "g_out_a": gain(ks[8], (DEPTH, A_WIDTH)),
        "g_out_b": gain(ks[9], (DEPTH, B_WIDTH)),
        "w_o": nrm(ks[10], (DEPTH, MIX_WIDTH, D_MODEL), MIX_WIDTH),
        "g_ffn": gain(ks[11], (DEPTH, D_MODEL)),
        "w_r1": nrm(ks[12], (DEPTH, D_MODEL, N_GROUPS), D_MODEL),
        "b_r1": 0.01 * jax.random.normal(ks[13], (DEPTH, N_GROUPS), f32),
        "w_r2": nrm(ks[14], (DEPTH, N_GROUPS, D_MODEL, EXPERTS_PER_GROUP), D_MODEL),
        "b_r2": 0.01 * jax.random.normal(ks[15], (DEPTH, N_GROUPS, EXPERTS_PER_GROUP), f32),
        "w_e_gate": nrm(ks[16], (DEPTH, N_EXPERTS, D_MODEL, EXPERT_FF), D_MODEL),
        "w_e_up": nrm(ks[17], (DEPTH, N_EXPERTS, D_MODEL, EXPERT_FF), D_MODEL),
        "w_e_down": nrm(ks[18], (DEPTH, N_EXPERTS, EXPERT_FF, D_MODEL), EXPERT_FF),
        "g_ple": gain(ks[19], (DEPTH, D_MODEL)),
        "w_ple_gate": nrm(ks[20], (DEPTH, D_MODEL, D_MODEL), D_MODEL),
        "w_ple_proj": nrm(ks[21], (DEPTH, PLE_DIM, D_MODEL), PLE_DIM),
        "g_final": gain(ks[22], (D_MODEL,)),
    }


def reference(x, p, g_mix, w_in, g_cq, w_uq, g_ckv, w_ukv, g_out_a, g_out_b, w_o,
              g_ffn, w_r1, b_r1, w_r2, b_r2, w_e_gate, w_e_up, w_e_down,
              g_ple, w_ple_gate, w_ple_proj, g_final):
    B, S, D = x.shape
    cos, sin = rope_tables(S, B_ROPE)
    h = x
    for i in range(DEPTH):
        a = rms_norm(h, g_mix[i])
        proj = a @ w_in[i]
        o = 0
        qa = proj[..., o:o + A_WIDTH].reshape(B, S, A_HEADS, A_HEAD_DIM); o += A_WIDTH
        ka = proj[..., o:o + A_WIDTH].reshape(B, S, A_HEADS, A_HEAD_DIM); o += A_WIDTH
        va = proj[..., o:o + A_WIDTH].reshape(B, S, A_HEADS, A_HEAD_DIM); o += A_WIDTH
        c_q = proj[..., o:o + Q_LORA]; o += Q_LORA
        c_kv = proj[..., o:o + KV_LORA]; o += KV_LORA
        k_rope = proj[..., o:o + B_ROPE]

        out_a = dilated_mixture_attention(qa, ka, va).reshape(B, S, A_WIDTH)

        q_b = (rms_norm(c_q, g_cq[i]) @ w_uq[i]).reshape(B, S, B_HEADS, B_NOPE + B_ROPE)
        q_nope = q_b[..., :B_NOPE]
        q_rope = apply_rope(q_b[..., B_NOPE:], cos[:, None, :], sin[:, None, :])
        kv_b = (rms_norm(c_kv, g_ckv[i]) @ w_ukv[i]).reshape(B, S, B_HEADS, B_NOPE + B_VDIM)
        k_nope = kv_b[..., :B_NOPE]
        v_b = kv_b[..., B_NOPE:]
        k_rope = apply_rope(k_rope, cos, sin)
        out_b = mla_attention(q_nope, q_rope, k_nope, k_rope, v_b)

        mixed = jnp.concatenate([rms_norm(out_a, g_out_a[i]), rms_norm(out_b, g_out_b[i])], axis=-1)
        h = h + mixed @ w_o[i]

        m = rms_norm(h, g_ffn[i]).reshape(B * S, D)
        h = h + hierarchical_moe(m, w_r1[i], b_r1[i], w_r2[i], b_r2[i],
                                 w_e_gate[i], w_e_up[i], w_e_down[i]).reshape(B, S, D)

        gate = jax.nn.sigmoid(rms_norm(h, g_ple[i]) @ w_ple_gate[i])
        h = h + gate * (p[i] @ w_ple_proj[i])
    return rms_norm(h, g_final)
```

```python
import numpy as np
from contextlib import ExitStack
import concourse.bass as bass
import concourse.mybir as mybir
from concourse.bass_utils import run_bass_kernel_spmd

F32 = mybir.dt.float32
BF16 = mybir.dt.bfloat16
ALU = mybir.AluOpType
AF = mybir.ActivationFunctionType
AX = mybir.AxisListType

S = 4096
D = 1024
NT = S // 128
EPS = 1e-6
NE = 32
BLK = 1024
NBLK = S // BLK
TPB = BLK // 128
SUB = 256
NSUB = BLK // SUB
DILS = (1, 4, 16)
import os
K_HPS = [int(c) for c in os.environ.get('K_HPS', '0123')]
K_BRS = [int(c) for c in os.environ.get('K_BRS', '012')]
MLA_SCALE = float(96 ** -0.5)


def _merge(d, t):
    for k, v in t.items():
        if d.get(k, 0) < v:
            d[k] = v


class Buf:
    def __init__(self, t, psum=False):
        self.t = t
        self.w = {}
        self.r = {}
        self.psum = psum

    def __getitem__(self, k):
        return self.t[k]


class Prog:
    ENG = ("pe", "act", "dve", "pool", "sp")

    def __init__(self, nc, es, n_dma_sems=32):
        self.nc = nc
        self.es = es
        self.ops = {k: [] for k in self.ENG}
        self.cnt = {k: 0 for k in self.ENG}
        self.waited = {k: {} for k in self.ENG}
        self.nsem = 0
        self.sem = {k: self.new_sem(k) for k in self.ENG}
        half = n_dma_sems // 2
        self.dsem = [self.new_sem("d") for _ in range(n_dma_sems)]
        self.dcnt = [0] * n_dma_sems
        self.dlast = [None] * n_dma_sems
        self.dring = {"sp": list(range(0, half)), "pool": list(range(half, n_dma_sems))}
        self.dpos = {"sp": 0, "pool": 0}
        self.pending = {k: False for k in self.ENG}

    def new_sem(self, name):
        self.nsem += 1
        return self.es.enter_context(self.nc.semaphore(f"s{name}{self.nsem}"))

    def _wait(self, eng, deps):
        for sem, val in deps.items():
            if sem is self.sem[eng] and val > self.cnt[eng]:
                assert eng == "pe"
                continue
            if self.waited[eng].get(sem, 0) < val:
                self.waited[eng][sem] = val
                self.ops[eng].append(lambda e, sem=sem, val=val: e.wait_ge(sem, val))

    def _deps(self, reads, writes, deps):
        d = {}
        for b in reads:
            _merge(d, b.w)
            if b.psum:
                _merge(d, b.r)
        for b in writes:
            _merge(d, b.w)
            _merge(d, b.r)
        if deps:
            _merge(d, deps)
        return d

    def _register(self, t, reads, writes, partial):
        for b in reads:
            _merge(b.r, t)
        for b in writes:
            if partial:
                _merge(b.w, t)
            else:
                b.w.clear()
                b.w.update(t)
            b.r.clear()

    def op(self, eng, fn, reads=(), writes=(), deps=None, partial=False, signal=True):
        self._wait(eng, self._deps(reads, writes, deps))
        if signal:
            self.cnt[eng] += 1
            sem, val = self.sem[eng], self.cnt[eng]
            self.ops[eng].append(lambda e: fn(e).then_inc(sem, 1))
            self.pending[eng] = False
            t = {sem: val}
            if self.cnt[eng] >= 30000:
                self.sem[eng] = self.new_sem(eng)
                self.cnt[eng] = 0
        else:
            self.ops[eng].append(lambda e: fn(e))
            self.pending[eng] = True
            t = {self.sem[eng]: self.cnt[eng] + 1}
        self._register(t, reads, writes, partial)
        return t

    def dma(self, q, out, in_, reads=(), writes=(), deps=None, partial=False, **kw):
        d = self._deps(reads, writes, deps)
        ring = self.dring[q]
        i = ring[self.dpos[q]]
        self.dpos[q] = (self.dpos[q] + 1) % len(ring)
        if self.dlast[i]:
            _merge(d, self.dlast[i])
        self._wait(q, d)
        self.dcnt[i] += 16
        sem, val = self.dsem[i], self.dcnt[i]
        self.ops[q].append(lambda e: e.dma_start(out=out, in_=in_, **kw).then_inc(sem, 16))
        t = {sem: val}
        self.dlast[i] = t
        self._register(t, reads, writes, partial)
        return t

    def emit(self, final_deps):
        nc = self.nc
        for k in self.ENG:
            if self.pending[k]:
                raise RuntimeError(f"engine {k} ends with unsignaled op")
        self._wait("sp", final_deps)
        with nc.Block() as block:
            @block.tensor
            def _(e):
                for f in self.ops["pe"]:
                    f(e)

            @block.scalar
            def _(e):
                for f in self.ops["act"]:
                    f(e)

            @block.vector
            def _(e):
                for f in self.ops["dve"]:
                    f(e)

            @block.gpsimd
            def _(e):
                for f in self.ops["pool"]:
                    f(e)

            @block.sync
            def _(e):
                for f in self.ops["sp"]:
                    f(e)


class Ring:
    def __init__(self, bufs):
        self.bufs = bufs
        self.i = 0

    def next(self):
        b = self.bufs[self.i]
        self.i = (self.i + 1) % len(self.bufs)
        return b


def build_program(upto="all", dbg=None):
    nc = bass.Bass("TRN2", target_bir_lowering=False)

    def din(name, shape, dt=F32):
        return nc.dram_tensor(name, list(shape), dt, kind="ExternalInput").ap()

    x_d = din("x", [S, D])
    p_d = din("p", [S, 256])
    w_in_d = din("w_in", [D, 2208])
    w_uq_d = din("w_uq", [384, 768])
    w_ukv_d = din("w_ukv", [256, 1024])
    w_o_d = din("w_o", [D, D])
    wr_d = din("wr", [D, 36])
    br_d = din("br", [36])
    weg_d = din("w_e_gate", [NE, D, 256])
    weu_d = din("w_e_up", [NE, D, 256])
    wed_d = din("w_e_down", [NE, 256, D])
    wpg_d = din("w_ple_gate", [D, D])
    wpp_d = din("w_ple_proj", [256, D])
    g_mix_d = din("g_mix", [D])
    g_cq_d = din("g_cq", [384])
    g_ckv_d = din("g_ckv", [256])
    g_out_d = din("g_out", [D])
    g_ffn_d = din("g_ffn", [D])
    g_ple_d = din("g_ple", [D])
    g_fin_d = din("g_final", [D])
    cos_d = din("cos_tm", [128, NT * 16])
    sin_d = din("sin_tm", [128, NT * 16])
    cos2_d = din("cos2_tm", [128, NT * 32])
    sin2_d = din("sin2_tm", [128, NT * 32])
    bias_d = din("bias_a", [128, 12 * 384])
    out_d = nc.dram_tensor("out", [S, D], F32, kind="ExternalOutput").ap()
    o_scr = nc.dram_tensor("o_scr", [S, D], F32, kind="Internal").ap()
    o_scr_b = Buf(None)
    cq_scr = nc.dram_tensor("cq_scr", [3, 128, S], F32, kind="Internal").ap()
    ckv_scr = nc.dram_tensor("ckv_scr", [2, 128, S], F32, kind="Internal").ap()
    kr_scr = nc.dram_tensor("kr_scr", [128, NT * 32], F32, kind="Internal").ap()
    lat_b = Buf(None)
    dbg_d = None
    if dbg is not None:
        dbg_d = nc.dram_tensor("dbg", list(dbg[1]), F32, kind="ExternalOutput").ap()

    def bc(ap1d, n):
        return ap1d.rearrange("(o d) -> o d", o=1).to_broadcast([128, n])

    final = {}
    with ExitStack() as es:
        P = Prog(nc, es)

        def snapshot():
            d = {}
            for k in P.ENG:
                if P.cnt[k] > 0:
                    d[P.sem[k]] = P.cnt[k]
            for i, sm in enumerate(P.dsem):
                if P.dcnt[i] > 0:
                    d[sm] = P.dcnt[i]
            return d

        def sb(name, shape, dt, stack=None):
            b = Buf((stack or es).enter_context(nc.sbuf_tensor(name, list(shape), dt)))
            b.w = snapshot()
            return b

        def ps(name, shape, dt, stack):
            b = Buf(stack.enter_context(nc.psum_tensor(name, list(shape), dt)), psum=True)
            b.w = snapshot()
            return b

        idb = sb("idb", [128, 128], BF16)
        idf = sb("idf", [128, 128], F32)
        for ident in (idb, idf):
            P.op("pool", lambda e, t=ident: e.memset(t[:], 1.0), writes=[ident])
            P.op("pool", lambda e, t=ident: e.affine_select(
                out=t[:], in_=t[:], pattern=[[-1, 128]], compare_op=ALU.is_equal,
                fill=0.0, base=0, channel_multiplier=1), writes=[ident])

        def rstd_from_ss(ss, rstd, n):
            P.op("act", lambda e: e.activation(out=ss[:], in_=ss[:], func=AF.Sqrt, scale=1.0 / n, bias=EPS),
                 writes=[ss])
            P.op("dve", lambda e: e.reciprocal(out=rstd[:], in_=ss[:]), reads=[ss], writes=[rstd])

        def dump(buf_ap, reads, shape_rows):
            t = P.dma("sp", dbg_d, buf_ap, reads=reads)
            _merge(final, t)

        esA = ExitStack()
        eW = ExitStack()
        esL = ExitStack()
        if True:
            aT = sb("aT", [128, 8, S], BF16, esA)
            wqkv = sb("wqkv", [128, 8, 1536], BF16, eW)
            w_in_v = w_in_d.rearrange("(c p) n -> p c n", p=128)
            for c in range(8):
                P.dma("pool", wqkv[:, c, :], w_in_v[:, c, 0:1536], writes=[wqkv], partial=True,
                      max_dma_last_dim=4096)
            biasA = sb("biasA", [128, 12, 384], BF16, eW)
            P.dma("pool", biasA[:].rearrange("p a b -> p (a b)"), bias_d, writes=[biasA],
                  max_dma_last_dim=4096)
            with ExitStack() as e1:
                gmix = sb("gmix", [128, D], F32, e1)
                P.dma("sp", gmix[:], bc(g_mix_d, D), writes=[gmix])
                xr = Ring([sb(f"x{i}", [128, D], F32, e1) for i in range(3)])
                sq = sb("sq", [128, D], F32, e1)
                abr = Ring([sb(f"ab{i}", [128, D], BF16, e1) for i in range(2)])
                ssr = Ring([sb(f"ss{i}", [128, 1], F32, e1) for i in range(2)])
                rsr = Ring([sb(f"rs{i}", [128, 1], F32, e1) for i in range(2)])
                tpr = Ring([ps(f"tp{i}", [128, 8, 128], BF16, e1) for i in range(2)])
                def p1_a(t):
                    xb = xr.next()
                    P.dma("sp", xb[:], x_d[t * 128:(t + 1) * 128, :], writes=[xb])
                    ss, rs, ab = ssr.next(), rsr.next(), abr.next()
                    P.op("act", lambda e: e.activation(out=sq[:], in_=xb[:], func=AF.Square, accum_out=ss[:]),
                         reads=[xb], writes=[sq, ss])
                    rstd_from_ss(ss, rs, D)
                    P.op("dve", lambda e: e.scalar_tensor_tensor(
                        out=ab[:], in0=xb[:], scalar=rs[:, 0:1], in1=gmix[:], op0=ALU.mult, op1=ALU.mult),
                        reads=[xb, rs, gmix], writes=[ab])
                    return ab

                def p1_b(t, ab):
                    tp = tpr.next()
                    for c in range(8):
                        P.op("pe", lambda e, c=c: e.transpose(tp[:, c, :], ab[:, c * 128:(c + 1) * 128], idb[:]),
                             reads=[ab, idb], writes=[tp], partial=True, signal=(c == 7))
                    P.op("act", lambda e: e.activation(out=aT[:, :, t * 128:(t + 1) * 128], in_=tp[:], func=AF.Copy),
                         reads=[tp], writes=[aT], partial=True)

                prev_ab = p1_a(0)
                for t in range(NT):
                    nxt_ab = p1_a(t + 1) if t + 1 < NT else None
                    p1_b(t, prev_ab)
                    prev_ab = nxt_ab
            if dbg and dbg[0] == "aT":
                stg = sb("dstg", [128, 8 * 256], F32, esA)
                P.op("dve", lambda e: e.tensor_copy(out=stg[:].rearrange("p (c t) -> p c t", c=8),
                                                    in_=aT[:, :, 0:256]), reads=[aT], writes=[stg])
                dump(stg[:], [stg], None)
            if upto == "p1":
                P.emit(final)
                return nc

            with ExitStack() as e3:
                QT = sb("QT", [128, S], BF16, e3)
                KT = sb("KT", [128, S], BF16, e3)
                QTd = {1: QT, 4: sb("QT4", [128, 4, S // 4], BF16, e3), 16: sb("QT16", [128, 16, S // 16], BF16, e3)}
                KTd = {1: KT, 4: sb("KT4", [128, 4, S // 4], BF16, e3), 16: sb("KT16", [128, 16, S // 16], BF16, e3)}
                Vp = sb("Vp", [128, NT, 2, 66], BF16, e3)
                P.op("pool", lambda e: e.memset(Vp[:], 1.0), writes=[Vp])
                Oacc = [sb(f"Oacc{i}", [65, S], F32, e3) for i in range(2)]
                oa_st = sb("oa_st", [128, NT, 128], F32, e3)
                PTr = Ring([sb(f"PT{i}", [128, 3, 128], BF16, e3) for i in range(3)])
                PRr = Ring([sb(f"PR{i}", [128, 3, 128], BF16, e3) for i in range(2)])
                rcr = Ring([sb(f"rc{i}", [128, 4, 1], F32, e3) for i in range(2)])
                pj = Ring([ps(f"pj{i}", [128, 512], F32, e3) for i in range(2)])
                scr_ = Ring([ps(f"sc{i}", [128, 3, 128], F32, e3) for i in range(2)])
                opr = Ring([ps(f"op{i}", [65, 128], F32, e3) for i in range(2)])
                otr = Ring([ps(f"ot{i}", [128, 4, 65], F32, e3) for i in range(2)])
                for hp in K_HPS:
                    for (dst, col0) in ((QT, hp * 128), (KT, 512 + hp * 128)):
                        for blk in range(8):
                            pp = pj.next()
                            for c in range(8):
                                P.op("pe", lambda e, pp=pp, c=c, col0=col0, blk=blk: e.matmul(
                                    pp[:], lhsT=wqkv[:, c, col0:col0 + 128],
                                    rhs=aT[:, c, blk * 512:(blk + 1) * 512], start=(c == 0), stop=(c == 7)),
                                    reads=[wqkv, aT], writes=[pp], partial=True, signal=(c == 7))
                            if dst is QT:
                                P.op("act", lambda e, pp=pp, blk=blk: e.activation(
                                    out=QT[:, blk * 512:(blk + 1) * 512], in_=pp[:], func=AF.Copy, scale=0.125),
                                    reads=[pp], writes=[QT], partial=True)
                                for d_ in (4, 16):
                                    P.op("act", lambda e, pp=pp, blk=blk, d_=d_: e.activation(
                                        out=QTd[d_][:, :, blk * 512 // d_:(blk + 1) * 512 // d_],
                                        in_=pp[:].rearrange("p (l r) -> p r l", r=d_), func=AF.Copy, scale=0.125),
                                        reads=[pp], writes=[QTd[d_]], partial=True)
                            else:
                                P.op("act", lambda e, pp=pp, blk=blk: e.activation(
                                    out=KT[:, blk * 512:(blk + 1) * 512], in_=pp[:], func=AF.Copy),
                                    reads=[pp], writes=[KT], partial=True)
                                for d_ in (4, 16):
                                    P.op("act", lambda e, pp=pp, blk=blk, d_=d_: e.activation(
                                        out=KTd[d_][:, :, blk * 512 // d_:(blk + 1) * 512 // d_],
                                        in_=pp[:].rearrange("p (l r) -> p r l", r=d_), func=AF.Copy),
                                        reads=[pp], writes=[KTd[d_]], partial=True)
                    for bi, dil in enumerate(DILS):
                        if bi not in K_BRS:
                            continue
                        L = S // dil
                        ntl = L // 128
                        for r in range(dil):
                            for j in range(ntl):
                                ch = r * ntl + j
                                t0 = j * 128 * dil + r
                                pp = pj.next()
                                for c in range(8):
                                    P.op("pe", lambda e, pp=pp, c=c, t0=t0, dil=dil, hp=hp: e.matmul(
                                        pp[:, 0:128], lhsT=aT[:, c, t0:t0 + 127 * dil + 1:dil],
                                        rhs=wqkv[:, c, 1024 + hp * 128:1024 + (hp + 1) * 128],
                                        start=(c == 0), stop=(c == 7)),
                                        reads=[wqkv, aT], writes=[pp], partial=True, signal=(c == 7))
                                P.op("act", lambda e, pp=pp, ch=ch: e.activation(
                                    out=Vp[:, ch, :, 0:64], in_=pp[:, 0:128].rearrange("p (h d) -> p h d", h=2),
                                    func=AF.Copy), reads=[pp], writes=[Vp], partial=True)
                        its = [(hh, r, i) for hh in range(2) for r in range(dil) for i in range(ntl)]

                        def a_score(it, bi=bi, dil=dil, ntl=ntl, hp=hp):
                            hh, r, i = it
                            h = hp * 2 + hh
                            bt = 3 - (-(h + 1) + 2 * bi)
                            pr0 = hh * 64
                            js = [j for j in (i - 1, i, i + 1) if 0 <= j < ntl]
                            sc = scr_.next()
                            q0 = i * 128 * dil + r
                            for n_, j in enumerate(js):
                                off = j - i + 1
                                k0 = j * 128 * dil + r
                                if dil == 1:
                                    lT_ = KT[pr0:pr0 + 64, j * 128:(j + 1) * 128]
                                    rh_ = QT[pr0:pr0 + 64, i * 128:(i + 1) * 128]
                                else:
                                    lT_ = KTd[dil][pr0:pr0 + 64, r, j * 128:(j + 1) * 128]
                                    rh_ = QTd[dil][pr0:pr0 + 64, r, i * 128:(i + 1) * 128]
                                P.op("pe", lambda e, sc=sc, off=off, lT_=lT_, rh_=rh_: e.matmul(
                                    sc[:, off, :], lhsT=lT_, rhs=rh_, start=True, stop=True),
                                    reads=[KTd[dil], QTd[dil]], writes=[sc], partial=True, signal=(n_ == len(js) - 1))
                            return (sc, js, q0, bt)

                        def a_exp(it, st_, nxt=None):
                            hh, r, i = it
                            sc, js, q0, bt = st_
                            o0 = js[0] - i + 1
                            o1 = js[-1] - i + 2
                            pr_, pt = PRr.next(), PTr.next()
                            P.op("act", lambda e: e.activation(out=pr_[:, o0:o1, :], in_=sc[:, o0:o1, :], func=AF.Exp),
                                 reads=[sc], writes=[pr_])
                            nst = a_score(nxt) if nxt is not None else None
                            P.op("dve", lambda e: e.tensor_tensor(
                                out=pt[:, o0:o1, :], in0=pr_[:, o0:o1, :],
                                in1=biasA[:, bt, o0 * 128:o1 * 128].rearrange("p (a b) -> p a b", b=128), op=ALU.mult),
                                reads=[pr_, biasA], writes=[pt])
                            return nst, (it, js, q0, pt)

                        def a_pv(it, js, q0, pt, bi=bi, dil=dil, ntl=ntl):
                            hh, r, i = it
                            opp = opr.next()
                            for n_, j in enumerate(js):
                                off = j - i + 1
                                ch = r * ntl + j
                                P.op("pe", lambda e, off=off, ch=ch, n_=n_, nj=len(js): e.matmul(
                                    opp[:], lhsT=Vp[:, ch, hh, 0:65], rhs=pt[:, off, :],
                                    start=(n_ == 0), stop=(n_ == nj - 1)),
                                    reads=[Vp, pt], writes=[opp], partial=True, signal=(n_ == len(js) - 1))
                            oa = Oacc[hh]
                            if bi == 0:
                                P.op("dve", lambda e: e.tensor_copy(out=oa[:, q0:q0 + 127 * dil + 1:dil], in_=opp[:]),
                                     reads=[opp], writes=[oa], partial=True)
                            else:
                                P.op("dve", lambda e: e.tensor_tensor(
                                    out=oa[:, q0:q0 + 127 * dil + 1:dil], in0=oa[:, q0:q0 + 127 * dil + 1:dil],
                                    in1=opp[:], op=ALU.add), reads=[opp], writes=[oa], partial=True)

                        st_ = a_score(its[0])
                        pv_pend = None
                        for n_it, it in enumerate(its):
                            st_, pv_new = a_exp(it, st_, nxt=(its[n_it + 1] if n_it + 1 < len(its) else None))
                            if pv_pend is not None:
                                a_pv(*pv_pend)
                            pv_pend = pv_new
                        a_pv(*pv_pend)
                    for hh in range(2):
                        oa = Oacc[hh]
                        for t4 in range(NT // 4):
                            ot = otr.next()
                            rc = rcr.next()
                            for k4 in range(4):
                                t = t4 * 4 + k4
                                P.op("pe", lambda e, ot=ot, oa=oa, t=t, k4=k4: e.transpose(
                                    ot[:, k4, :], oa[:, t * 128:(t + 1) * 128], idf[0:65, 0:65]),
                                    reads=[oa, idf], writes=[ot], partial=True, signal=(k4 == 3))
                            P.op("dve", lambda e, ot=ot, rc=rc: e.reciprocal(out=rc[:], in_=ot[:, :, 64:65]),
                                 reads=[ot], writes=[rc])
                            for k4 in range(4):
                                t = t4 * 4 + k4
                                P.op("dve", lambda e, ot=ot, rc=rc, t=t, hh=hh, k4=k4: e.tensor_scalar(
                                    out=oa_st[:, t, hh * 64:(hh + 1) * 64], in0=ot[:, k4, 0:64], scalar1=rc[:, k4, 0:1],
                                    scalar2=None, op0=ALU.mult), reads=[ot, rc], writes=[oa_st], partial=True)
                    for q4 in range(4):
                        t = P.dma("sp", o_scr.rearrange("(t p) d -> p t d", p=128)[:, q4 * 8:(q4 + 1) * 8,
                                                                                hp * 128:(hp + 1) * 128],
                                  oa_st[:, q4 * 8:(q4 + 1) * 8, :], reads=[oa_st], writes=[o_scr_b], partial=True)
                        _merge(final, t)
                if dbg and dbg[0] == "wdump":
                    _merge(final, P.dma("pool", dbg_d[:, 0:1536], wqkv[:, 0, :], reads=[wqkv]))
                    _merge(final, P.dma("pool", dbg_d[:, 1536:3072], wqkv[:, 7, :], reads=[wqkv]))
                    _merge(final, P.dma("pool", dbg_d[:, 3072:3072 + 384], biasA[:, 11, :], reads=[biasA]))
                if dbg and dbg[0] == "p3dump":
                    _merge(final, P.dma("pool", dbg_d[:, 0:512], QT[:, 0:512], reads=[QT]))
                    _merge(final, P.dma("pool", dbg_d[:, 512:1024], KT[:, 0:512], reads=[KT]))
                    _merge(final, P.dma("pool", dbg_d[:, 1024:3136], Vp[:, 0:16, :, :].rearrange("p a b c -> p (a b c)"), reads=[Vp]))
                    _merge(final, P.dma("sp", dbg_d[0:65, 3136:3648], Oacc[0][:, 0:512], reads=[Oacc[0]]))
            if dbg and dbg[0] == "o_scr_a":
                _merge(final, P.dma("sp", dbg_d, o_scr[:, 0:512], reads=[o_scr_b]))
            if upto == "p3":
                P.emit(final)
                return nc

            eW.close()
            cqT = sb("cqT", [128, 3, S], BF16, esL)
            ckvT = sb("ckvT", [128, 2, S], BF16, esL)
            krt = sb("krt", [128, NT, 32], BF16, esL)
            with ExitStack() as e2:
                wlat = sb("wlat", [128, 8, 672], BF16, e2)
                for c in range(8):
                    P.dma("pool", wlat[:, c, :], w_in_v[:, c, 1536:2208], writes=[wlat], partial=True)
                gcq = sb("gcq", [128, 384], F32, e2)
                gckv = sb("gckv", [128, 256], F32, e2)
                P.dma("sp", gcq[:], bc(g_cq_d, 384), writes=[gcq])
                P.dma("sp", gckv[:], bc(g_ckv_d, 256), writes=[gckv])
                cosb = sb("cosb", [128, NT, 16], F32, e2)
                sinb = sb("sinb", [128, NT, 16], F32, e2)
                P.dma("sp", cosb[:].rearrange("p t f -> p (t f)"), cos_d, writes=[cosb])
                P.dma("sp", sinb[:].rearrange("p t f -> p (t f)"), sin_d, writes=[sinb])
                LAr = Ring([ps(f"LA{i}", [128, 384], F32, e2) for i in range(2)])
                LBr = Ring([ps(f"LB{i}", [128, 288], F32, e2) for i in range(2)])
                tqr = Ring([ps(f"tq{i}", [128, 5, 128], BF16, e2) for i in range(2)])
                sq2 = sb("sq2", [128, 384], F32, e2)
                cqr = Ring([sb(f"cqn{i}", [128, 640], BF16, e2) for i in range(2)])
                tmr = Ring([sb(f"tm{i}", [128, 64], F32, e2) for i in range(2)])
                s4r = Ring([sb(f"s4{i}", [128, 4], F32, e2) for i in range(2)])
                def p2_a(t):
                    LA, LB, tq, cqn, tm, s4 = LAr.next(), LBr.next(), tqr.next(), cqr.next(), tmr.next(), s4r.next()
                    tk = slice(t * 128, (t + 1) * 128)
                    for (dst, c0, c1) in ((LA, 0, 384), (LB, 384, 672)):
                        for c in range(8):
                            P.op("pe", lambda e, dst=dst, c=c, c0=c0, c1=c1, tk=tk: e.matmul(
                                dst[:], lhsT=aT[:, c, tk], rhs=wlat[:, c, c0:c1], start=(c == 0), stop=(c == 7)),
                                reads=[aT, wlat], writes=[dst], partial=True, signal=(c == 7))
                    ssq, sskv, rq, rkv = (Buf(s4.t[:, i:i + 1]) for i in range(4))
                    for b_ in (ssq, sskv, rq, rkv):
                        b_.w = dict(s4.w); b_.r = dict(s4.r)
                    P.op("act", lambda e, LA=LA, ssq=ssq: e.activation(out=sq2[:, 0:384], in_=LA[:], func=AF.Square,
                                                                      accum_out=ssq.t), reads=[LA], writes=[sq2, ssq])
                    P.op("act", lambda e, LB=LB, sskv=sskv: e.activation(out=sq2[:, 0:256], in_=LB[:, 0:256], func=AF.Square,
                                                                        accum_out=sskv.t), reads=[LB], writes=[sq2, sskv])
                    for (ss_, rs_, n_) in ((ssq, rq, 384), (sskv, rkv, 256)):
                        P.op("act", lambda e, ss_=ss_, n_=n_: e.activation(out=ss_.t, in_=ss_.t, func=AF.Sqrt,
                                                                          scale=1.0 / n_, bias=EPS), writes=[ss_])
                        P.op("dve", lambda e, ss_=ss_, rs_=rs_: e.reciprocal(out=rs_.t, in_=ss_.t), reads=[ss_], writes=[rs_])
                    P.op("dve", lambda e, LA=LA, rq=rq, cqn=cqn: e.scalar_tensor_tensor(
                        out=cqn[:, 0:384], in0=LA[:], scalar=rq.t, in1=gcq[:], op0=ALU.mult, op1=ALU.mult),
                        reads=[LA, rq, gcq], writes=[cqn], partial=True)
                    P.op("dve", lambda e, LB=LB, rkv=rkv, cqn=cqn: e.scalar_tensor_tensor(
                        out=cqn[:, 384:640], in0=LB[:, 0:256], scalar=rkv.t, in1=gckv[:], op0=ALU.mult, op1=ALU.mult),
                        reads=[LB, rkv, gckv], writes=[cqn], partial=True)
                    _merge(s4.r, rq.r); _merge(s4.r, rkv.r); _merge(s4.r, ssq.r); _merge(s4.r, sskv.r)
                    _merge(s4.w, rq.w); _merge(s4.w, rkv.w); _merge(s4.w, ssq.w); _merge(s4.w, sskv.w)
                    for (k_, xa, tb) in ((0, 256, cosb), (1, 272, sinb), (2, 256, sinb), (3, 272, cosb)):
                        P.op("dve", lambda e, LB=LB, tm=tm, k_=k_, xa=xa, tb=tb, t=t: e.tensor_tensor(
                            out=tm[:, k_ * 16:(k_ + 1) * 16], in0=LB[:, xa:xa + 16], in1=tb[:, t, :], op=ALU.mult),
                            reads=[LB, tb], writes=[tm], partial=True)
                    P.op("dve", lambda e, tm=tm, t=t: e.tensor_tensor(out=krt[:, t, 0:16], in0=tm[:, 0:16], in1=tm[:, 16:32],
                                                                     op=ALU.subtract), reads=[tm], writes=[krt], partial=True)
                    P.op("dve", lambda e, tm=tm, t=t: e.tensor_tensor(out=krt[:, t, 16:32], in0=tm[:, 32:48], in1=tm[:, 48:64],
                                                                     op=ALU.add), reads=[tm], writes=[krt], partial=True)
                    return (tq, cqn, tk)

                def p2_b(t, tq, cqn, tk):
                    for c in range(5):
                        P.op("pe", lambda e, tq=tq, cqn=cqn, c=c: e.transpose(tq[:, c, :], cqn[:, c * 128:(c + 1) * 128], idb[:]),
                             reads=[cqn, idb], writes=[tq], partial=True, signal=(c == 4))
                    P.op("act", lambda e, tq=tq, tk=tk: e.activation(out=cqT[:, :, tk], in_=tq[:, 0:3, :], func=AF.Copy),
                         reads=[tq], writes=[cqT], partial=True)
                    P.op("act", lambda e, tq=tq, tk=tk: e.activation(out=ckvT[:, :, tk], in_=tq[:, 3:5, :], func=AF.Copy),
                         reads=[tq], writes=[ckvT], partial=True)

                prev2 = p2_a(0)
                for t in range(NT):
                    nxt2 = p2_a(t + 1) if t + 1 < NT else None
                    p2_b(t, *prev2)
                    prev2 = nxt2
                if dbg and dbg[0] == "lat":
                    for c in range(3):
                        _merge(final, P.dma("pool", dbg_d[:, c * 4096:(c + 1) * 4096], cqT[:, c, :], reads=[cqT]))
            if upto == "p2":
                P.emit(final)
                return nc

        with ExitStack() as esM:
            wuq = sb("wuq", [128, 3, 768], BF16, esM)
            wukv = sb("wukv", [128, 2, 1024], BF16, esM)
            P.dma("pool", wuq[:], w_uq_d.rearrange("(c p) n -> p c n", p=128), writes=[wuq])
            P.dma("pool", wukv[:], w_ukv_d.rearrange("(c p) n -> p c n", p=128), writes=[wukv])
            cos2 = sb("cos2", [128, NT, 2, 16], F32, esM)
            sin2 = sb("sin2", [128, NT, 2, 16], F32, esM)
            P.dma("sp", cos2[:].rearrange("p t h f -> p (t h f)"), cos2_d, writes=[cos2])
            P.dma("sp", sin2[:].rearrange("p t h f -> p (t h f)"), sin2_d, writes=[sin2])
            QT2 = sb("QT2", [128, 2, S], BF16, esM)
            KT2 = sb("KT2", [128, 2, S], BF16, esM)
            P.op("pool", lambda e: e.memset(QT2[:], 0.0), writes=[QT2])
            P.op("pool", lambda e: e.memset(KT2[:], 0.0), writes=[KT2])
            Vp2 = sb("Vp2", [128, NT, 2, 66], BF16, esM)
            P.op("pool", lambda e: e.memset(Vp2[:], 1.0), writes=[Vp2])
            ob_st = sb("ob_st", [128, NT, 128], F32, esM)
            Gm = ps("Gm", [128, 512], F32, esM)

            def view(ap):
                v = Buf(ap, psum=True)
                v.w, v.r = Gm.w, Gm.r
                return v
            Qp_r = Ring([view(Gm.t[:, 0:192].rearrange("p (h d) -> p h d", h=2))])
            KVp_r = Ring([view(Gm.t[:, 256:512].rearrange("p (h d) -> p h d", h=2))])
            ot_r = Ring([view(Gm.t[:, 0:260].rearrange("p (k d) -> p k d", k=4))])
            tQK_r = Ring([ps(f"tQK{i}", [128, 4, 128], BF16, esM) for i in range(1)])
            St_r = Ring([ps(f"St{i}", [128, 1024], F32, esM) for i in range(2)])
            O_r = Ring([ps(f"Ops{i}", [65, 512], F32, esM) for i in range(2)])
            Qtm_r = Ring([sb(f"Qtm{i}", [128, 2, 96], BF16, esM) for i in range(2)])
            Ktm_r = Ring([sb(f"Ktm{i}", [128, 2, 96], BF16, esM) for i in range(2)])
            tm2_r = Ring([sb(f"tm2{i}", [128, 4, 2, 16], F32, esM) for i in range(2)])
            PT2_r = Ring([sb(f"PTb{i}", [128, 1024], BF16, esM) for i in range(3)])
            Osb_r = Ring([sb(f"Osb{i}", [65, 512], F32, esM) for i in range(2)])
            rc2_r = Ring([sb(f"rcb{i}", [128, 4, 1], F32, esM) for i in range(2)])
            def mb_s1(hp, t):
                tk = slice(t * 128, (t + 1) * 128)
                Qps, KVps, Qtm, Ktm, tm2 = Qp_r.next(), KVp_r.next(), Qtm_r.next(), Ktm_r.next(), tm2_r.next()
                for c in range(3):
                    P.op("pe", lambda e, c=c: e.matmul(
                        Qps[:].rearrange("p h d -> p (h d)"), lhsT=cqT[:, c, tk], rhs=wuq[:, c, hp * 192:(hp + 1) * 192],
                        start=(c == 0), stop=(c == 2)), reads=[cqT, wuq], writes=[Qps], partial=True, signal=(c == 2))
                for c in range(2):
                    P.op("pe", lambda e, c=c: e.matmul(
                        KVps[:].rearrange("p h d -> p (h d)"), lhsT=ckvT[:, c, tk], rhs=wukv[:, c, hp * 256:(hp + 1) * 256],
                        start=(c == 0), stop=(c == 1)), reads=[ckvT, wukv], writes=[KVps], partial=True, signal=(c == 1))
                P.op("dve", lambda e: e.tensor_copy(out=Qtm[:, :, 0:64], in_=Qps[:, :, 0:64]),
                     reads=[Qps], writes=[Qtm], partial=True)
                P.op("dve", lambda e: e.tensor_copy(out=Ktm[:, :, 0:64], in_=KVps[:, :, 0:64]),
                     reads=[KVps], writes=[Ktm], partial=True)
                P.op("dve", lambda e: e.tensor_copy(out=Vp2[:, t, :, 0:64], in_=KVps[:, :, 64:128]),
                     reads=[KVps], writes=[Vp2], partial=True)
                for (k_, xa, tb) in ((0, 64, cos2), (1, 80, sin2), (2, 64, sin2), (3, 80, cos2)):
                    P.op("dve", lambda e, k_=k_, xa=xa, tb=tb: e.tensor_tensor(
                        out=tm2[:, k_, :, :], in0=Qps[:, :, xa:xa + 16], in1=tb[:, t, :, :], op=ALU.mult),
                        reads=[Qps, tb], writes=[tm2], partial=True)
                P.op("dve", lambda e: e.tensor_tensor(out=Qtm[:, :, 64:80], in0=tm2[:, 0, :, :], in1=tm2[:, 1, :, :],
                                                     op=ALU.subtract), reads=[tm2], writes=[Qtm], partial=True)
                P.op("dve", lambda e: e.tensor_tensor(out=Qtm[:, :, 80:96], in0=tm2[:, 2, :, :], in1=tm2[:, 3, :, :],
                                                     op=ALU.add), reads=[tm2], writes=[Qtm], partial=True)
                for hh in range(2):
                    P.op("pool", lambda e, hh=hh: e.tensor_copy(out=Ktm[:, hh, 64:96], in_=krt[:, t, :]),
                         reads=[krt], writes=[Ktm], partial=True)
                return (Qtm, Ktm)

            def mb_s2(t, Qtm, Ktm):
                tk = slice(t * 128, (t + 1) * 128)
                tQK = tQK_r.next()
                for hh in range(2):
                    P.op("pe", lambda e, hh=hh: e.transpose(tQK[0:96, hh, :], Qtm[:, hh, :], idb[:]),
                         reads=[Qtm, idb], writes=[tQK], partial=True, signal=False)
                for hh in range(2):
                    P.op("pe", lambda e, hh=hh: e.transpose(tQK[0:96, 2 + hh, :], Ktm[:, hh, :], idb[:]),
                         reads=[Ktm, idb], writes=[tQK], partial=True, signal=(hh == 1))
                P.op("act", lambda e: e.activation(out=QT2[0:96, :, tk], in_=tQK[0:96, 0:2, :], func=AF.Copy),
                     reads=[tQK], writes=[QT2], partial=True)
                P.op("act", lambda e: e.activation(out=KT2[0:96, :, tk], in_=tQK[0:96, 2:4, :], func=AF.Copy),
                     reads=[tQK], writes=[KT2], partial=True)

            for hp in K_HPS:
                prev = mb_s1(hp, 0)
                for t in range(NT):
                    nxt_ = mb_s1(hp, t + 1) if t + 1 < NT else None
                    mb_s2(t, *prev)
                    prev = nxt_
                NP_ = NT // 2
                steps = [(hh, qb, kp) for hh in range(int(os.environ.get('K_P4H', 2)))
                         for qb in range(int(os.environ.get('K_P4Q', 8))) for kp in range(NP_)]
                ops_of = {}

                def b_score(step):
                    hh, qb, kp = step
                    qs = slice(qb * 512, (qb + 1) * 512)
                    St = St_r.next()
                    for j2 in range(2):
                        kc = 2 * kp + j2
                        P.op("pe", lambda e, kc=kc, j2=j2: e.matmul(
                            St[:, j2 * 512:(j2 + 1) * 512], lhsT=KT2[:, hh, kc * 128:(kc + 1) * 128], rhs=QT2[:, hh, qs],
                            start=True, stop=True),
                            reads=[KT2, QT2], writes=[St], partial=True, signal=(j2 == 1))
                    return St

                def b_pv(step, PT):
                    hh, qb, kp = step
                    if kp == 0:
                        ops_of[(hh, qb)] = O_r.next()
                    Ops = ops_of[(hh, qb)]
                    for j2 in range(2):
                        kc = 2 * kp + j2
                        P.op("pe", lambda e, kc=kc, j2=j2: e.matmul(
                            Ops[:], lhsT=Vp2[:, kc, hh, 0:65], rhs=PT[:, j2 * 512:(j2 + 1) * 512],
                            start=(kc == 0), stop=(kc == NT - 1)),
                            reads=[Vp2, PT], writes=[Ops], partial=True, signal=(kc == NT - 1))
                    if kp != NP_ - 1:
                        return
                    Osb = Osb_r.next()
                    P.op("dve", lambda e: e.tensor_copy(out=Osb[:], in_=Ops[:]), reads=[Ops], writes=[Osb])
                    ot, rc = ot_r.next(), rc2_r.next()
                    for k4 in range(4):
                        P.op("pe", lambda e, k4=k4: e.transpose(
                            ot[:, k4, :], Osb[:, k4 * 128:(k4 + 1) * 128], idf[0:65, 0:65]),
                            reads=[Osb, idf], writes=[ot], partial=True, signal=(k4 == 3))
                    P.op("dve", lambda e: e.reciprocal(out=rc[:], in_=ot[:, :, 64:65]), reads=[ot], writes=[rc])
                    for k4 in range(4):
                        t = qb * 4 + k4
                        P.op("dve", lambda e, t=t, k4=k4: e.tensor_scalar(
                            out=ob_st[:, t, hh * 64:(hh + 1) * 64], in0=ot[:, k4, 0:64], scalar1=rc[:, k4, 0:1], scalar2=None,
                            op0=ALU.mult), reads=[ot, rc], writes=[ob_st], partial=True)

                if steps:
                    St_next = b_score(steps[0])
                    pv_pend = None
                    for n_s, step in enumerate(steps):
                        St, PT = St_next, PT2_r.next()
                        P.op("act", lambda e, St=St, PT=PT: e.activation(out=PT[:], in_=St[:], func=AF.Exp, scale=MLA_SCALE),
                             reads=[St], writes=[PT])
                        if n_s + 1 < len(steps):
                            St_next = b_score(steps[n_s + 1])
                        if pv_pend is not None:
                            b_pv(*pv_pend)
                        pv_pend = (step, PT)
                    b_pv(*pv_pend)
                for q4 in range(4):
                    t_ = P.dma("sp", o_scr.rearrange("(t p) d -> p t d", p=128)[:, q4 * 8:(q4 + 1) * 8,
                                                                             512 + hp * 128:512 + (hp + 1) * 128],
                               ob_st[:, q4 * 8:(q4 + 1) * 8, :], reads=[ob_st], writes=[o_scr_b], partial=True)
                    _merge(final, t_)
            if dbg and dbg[0] == "p4in":
                _merge(final, P.dma("pool", dbg_d[:, 0:4096], cqT[:, 2, :], reads=[cqT]))
                _merge(final, P.dma("pool", dbg_d[:, 4096:4864], wuq[:, 2, :], reads=[wuq]))
                _merge(final, P.dma("pool", dbg_d[:, 4864:5888], wukv[:, 1, :], reads=[wukv]))
                _merge(final, P.dma("pool", dbg_d[:, 5888:6912], krt[:].rearrange("p t f -> p (t f)"), reads=[krt]))
            if dbg and dbg[0] == "p4dump":
                _merge(final, P.dma("pool", dbg_d[:, 0:512], QT2[:, 0, 0:512], reads=[QT2]))
                _merge(final, P.dma("pool", dbg_d[:, 512:1024], KT2[:, 0, 0:512], reads=[KT2]))
                _merge(final, P.dma("pool", dbg_d[:, 1024:3136], Vp2[:, 0:16, :, :].rearrange("p a b c -> p (a b c)"), reads=[Vp2]))
                _merge(final, P.dma("sp", dbg_d[0:65, 3136:3648], Osb_r.bufs[0][:, :], reads=[Osb_r.bufs[0]]))
            if dbg and dbg[0] == "o_scr":
                _merge(final, P.dma("sp", dbg_d, o_scr, reads=[o_scr_b]))
        esL.close()
        esA.close()
        if upto == "p4":
            P.emit(final)
            return nc

        with ExitStack() as esF:
            wo = sb("wo", [128, 8, D], BF16, esF)
            wpg = sb("wpg", [128, 8, D], BF16, esF)
            wpp = sb("wpp", [128, 2, D], BF16, esF)
            for c in range(8):
                P.dma("pool", wo[:, c, :], w_o_d[c * 128:(c + 1) * 128, :], writes=[wo], partial=True, max_dma_last_dim=4096)
            wrf = sb("wrf", [128, 8, 36], F32, esF)
            P.dma("sp", wrf[:], wr_d.rearrange("(c p) n -> p c n", p=128), writes=[wrf])
            wrh = sb("wrh", [128, 8, 36], BF16, esF)
            wrl = sb("wrl", [128, 8, 36], BF16, esF)
            P.op("dve", lambda e: e.tensor_copy(out=wrh[:], in_=wrf[:]), reads=[wrf], writes=[wrh])
            P.op("dve", lambda e: e.tensor_tensor(out=wrl[:], in0=wrf[:], in1=wrh[:], op=ALU.subtract), reads=[wrf, wrh], writes=[wrl])
            gains = {}
            for nm, gd in (("gout", g_out_d), ("gffn", g_ffn_d), ("gple", g_ple_d), ("gfin", g_fin_d)):
                gains[nm] = sb(nm, [128, D], F32, esF)
                P.dma("sp", gains[nm][:], bc(gd, D), writes=[gains[nm]])
            gout, gffn, gple, gfin = gains["gout"], gains["gffn"], gains["gple"], gains["gfin"]
            brb = sb("brb", [128, 36], F32, esF)
            P.dma("sp", brb[:], bc(br_d, 36), writes=[brb])
            for c in range(8):
                P.dma("pool", wpg[:, c, :], wpg_d[c * 128:(c + 1) * 128, :], writes=[wpg], partial=True, max_dma_last_dim=4096)
            for c in range(2):
                P.dma("pool", wpp[:, c, :], wpp_d[c * 128:(c + 1) * 128, :], writes=[wpp], partial=True, max_dma_last_dim=4096)
            yacc = [sb(f"yacc{i}", [128, D], F32, esF) for i in range(TPB)]
            mTb = sb("mTb", [128, 8, BLK], BF16, esF)
            cw_one = sb("cw", [128, TPB, 32], F32, esF)
            cw = [cw_one] * NBLK
            PS = [ps(f"PS{i}", [128, 1024], F32, esF) for i in range(4)]
            psr = Ring(PS)
            gu_r = Ring(PS[0:2])
            y_r = Ring(PS[2:4])
            NSLOT = 3
            SL = []
            for s_ in range(NSLOT):
                ox_ = sb(f"oxtile{s_}", [128, D], F32, esF)
                SL.append(dict(
                    f32=Ring([sb(f"f32_{s_}_{i}", [128, D], F32, esF) for i in range(3)]),
                    o=ox_, x=ox_,
                    p=sb(f"ptile{s_}", [128, 256], F32, esF), bfT=sb(f"bfT{s_}", [128, 8, 128], BF16, esF),
                    pT=sb(f"pT{s_}", [128, 2, 128], BF16, esF), rt=sb(f"rt{s_}", [128, 96], F32, esF),
                    mlo=sb(f"mlo{s_}", [128, 8, 128], BF16, esF), ps=PS[s_]))
            sqs = sb("sqs", [128, D], BF16, esF)
            sgt_r = Ring([sb(f"sgt{i}", [128, 512], F32, esF) for i in range(2)])
            hid_r = Ring([sb(f"hid{i}", [128, 512], BF16, esF) for i in range(2)])
            ss_r = Ring([sb(f"fss{i}", [128, 1], F32, esF) for i in range(12)])
            rs_r = Ring([sb(f"frs{i}", [128, 1], F32, esF) for i in range(12)])
            wg_r = Ring([sb(f"wg{i}", [128, 8, 256], BF16, esF) for i in range(2)])
            wu_r = Ring([sb(f"wu{i}", [128, 8, 256], BF16, esF) for i in range(2)])
            wd_r = Ring([sb(f"wd{i}", [128, 2, D], BF16, esF) for i in range(2)])

            def rstd_of(src, reads, n):
                ss, rs = ss_r.next(), rs_r.next()
                P.op("act", lambda e: e.activation(out=sqs[:, 0:n], in_=src, func=AF.Square, accum_out=ss[:]),
                     reads=reads, writes=[sqs, ss])
                rstd_from_ss(ss, rs, n)
                return rs

            def transpose8(dst, src, n=8):
                for c in range(n):
                    P.op("pe", lambda e, c=c: e.transpose(dst[:, c * 128:(c + 1) * 128], src[:, c * 128:(c + 1) * 128], idf[:]),
                         reads=[src, idf], writes=[dst], partial=True, signal=(c == n - 1))

            def mm16(dst, lT, w_, nck=8):
                for half in range(2):
                    for c in range(nck):
                        P.op("pe", lambda e, half=half, c=c: e.matmul(
                            dst[:, half * 512:(half + 1) * 512], lhsT=lT[:, c, :], rhs=w_[:, c, half * 512:(half + 1) * 512],
                            start=(c == 0), stop=(c == nck - 1)),
                            reads=[lT, w_], writes=[dst], partial=True, signal=(half == 1 and c == nck - 1))

            def load_expert(e_):
                wg, wu, wd = wg_r.next(), wu_r.next(), wd_r.next()
                P.dma("pool", wg[:], weg_d[e_].rearrange("(c p) n -> p c n", p=128), writes=[wg])
                P.dma("pool", wu[:], weu_d[e_].rearrange("(c p) n -> p c n", p=128), writes=[wu])
                P.dma("pool", wd[:], wed_d[e_].rearrange("(c p) n -> p c n", p=128), writes=[wd])
                return (wg, wu, wd)

            W_next = {}
            n_blk = int(os.environ.get("K_NBLK", NBLK))
            n_exp = int(os.environ.get("K_NEXP", NE))
            def p5_tile(b, tl, sl):
                t = b * TPB + tl
                rows = slice(t * 128, (t + 1) * 128)
                ot_, xt = sl["o"], sl["x"]
                P.dma("sp", ot_[:], o_scr[rows, :], reads=[o_scr_b], writes=[ot_])
                yield
                ra = rstd_of(ot_[:, 0:512], [ot_], 512)
                yield
                rb = rstd_of(ot_[:, 512:1024], [ot_], 512)
                yield
                mx = sl["f32"].next()
                for (r_, lo) in ((ra, 0), (rb, 512)):
                    P.op("dve", lambda e, r_=r_, lo=lo, ot_=ot_, mx=mx: e.scalar_tensor_tensor(
                        out=mx[:, lo:lo + 512], in0=ot_[:, lo:lo + 512], scalar=r_[:, 0:1], in1=gout[:, lo:lo + 512],
                        op0=ALU.mult, op1=ALU.mult), reads=[ot_, r_, gout], writes=[mx], partial=True)
                P.dma("sp", xt[:], x_d[rows, :], writes=[xt])
                yield
                pa = sl["ps"]
                transpose8(pa, mx)
                yield
                mixT = sl["bfT"]
                P.op("act", lambda e, pa=pa, mixT=mixT: e.activation(
                    out=mixT[:], in_=pa[:].rearrange("p (c t) -> p c t", c=8), func=AF.Copy), reads=[pa], writes=[mixT])
                yield
                ph = sl["ps"]
                mm16(ph, mixT, wo)
                yield
                ya = yacc[tl]
                P.op("dve", lambda e, ph=ph, xt=xt, ya=ya: e.tensor_tensor(out=ya[:], in0=ph[:], in1=xt[:], op=ALU.add),
                     reads=[ph, xt], writes=[ya])
                yield
                if dbg and dbg[0] == "h1":
                    _merge(final, P.dma("sp", dbg_d[rows, :], ya[:], reads=[ya]))
                rm = rstd_of(ya[:], [ya], D)
                yield
                mf = sl["f32"].next()
                P.op("dve", lambda e, ya=ya, rm=rm, mf=mf: e.scalar_tensor_tensor(
                    out=mf[:], in0=ya[:], scalar=rm[:, 0:1], in1=gffn[:], op0=ALU.mult, op1=ALU.mult),
                    reads=[ya, rm, gffn], writes=[mf])
                yield
                pm = sl["ps"]
                transpose8(pm, mf)
                yield
                mTf = sl["f32"].next()
                P.op("act", lambda e, pm=pm, mTf=mTf: e.activation(out=mTf[:], in_=pm[:], func=AF.Copy), reads=[pm], writes=[mTf])
                yield
                P.op("dve", lambda e, pm=pm, tl=tl: e.tensor_copy(
                    out=mTb[:, :, tl * 128:(tl + 1) * 128], in_=pm[:].rearrange("p (c t) -> p c t", c=8)),
                    reads=[pm], writes=[mTb], partial=True)
                yield
                mlo = sl["mlo"]
                P.op("dve", lambda e, mTf=mTf, mlo=mlo, tl=tl: e.tensor_tensor(
                    out=mlo[:], in0=mTf[:].rearrange("p (c t) -> p c t", c=8), in1=mTb[:, :, tl * 128:(tl + 1) * 128],
                    op=ALU.subtract), reads=[mTf, mTb], writes=[mlo])
                yield
                pr = sl["ps"]
                k = 0
                for (lt_, w_) in (("hi", wrh), ("lo", wrh), ("hi", wrl)):
                    for c in range(8):
                        k += 1
                        P.op("pe", lambda e, pr=pr, mlo=mlo, c=c, lt_=lt_, w_=w_, tl=tl, k=k: e.matmul(
                            pr[:, 0:36], lhsT=(mTb[:, c, tl * 128:(tl + 1) * 128] if lt_ == "hi" else mlo[:, c, :]),
                            rhs=w_[:, c, :], start=(k == 1), stop=(k == 24)),
                            reads=[mTb, mlo, w_], writes=[pr], partial=True, signal=(k == 24))
                rt = sl["rt"]
                cwb = cw[b]

                def R(eng, fn, extra_r=(), extra_w=()):
                    P.op(eng, fn, reads=list(extra_r), writes=[rt] + list(extra_w), partial=True)
                R("dve", lambda e, pr=pr, rt=rt: e.tensor_tensor(out=rt[:, 0:36], in0=pr[:, 0:36], in1=brb[:], op=ALU.add), [pr, brb])
                yield
                R("dve", lambda e, rt=rt: e.tensor_reduce(out=rt[:, 36:37], in_=rt[:, 0:4], axis=AX.X, op=ALU.max))
                yield
                R("dve", lambda e, rt=rt: e.tensor_scalar(out=rt[:, 37:38], in0=rt[:, 36:37], scalar1=-1.0, scalar2=None, op0=ALU.mult))
                yield
                R("act", lambda e, rt=rt: e.activation(out=rt[:, 88:92], in_=rt[:, 0:4], func=AF.Exp, bias=rt[:, 37:38],
                                                      accum_out=rt[:, 38:39]))
                yield
                R("dve", lambda e, rt=rt: e.reciprocal(out=rt[:, 39:40], in_=rt[:, 38:39]))
                yield
                R("dve", lambda e, rt=rt: e.tensor_scalar(out=rt[:, 40:44], in0=rt[:, 0:4], scalar1=rt[:, 36:37], scalar2=None,
                                                         op0=ALU.is_equal))
                yield
                R("dve", lambda e, rt=rt: e.tensor_scalar(out=rt[:, 44:52], in0=rt[:, 4:12], scalar1=rt[:, 40:41], scalar2=None,
                                                         op0=ALU.mult))
                yield
                for g_ in range(1, 4):
                    R("dve", lambda e, rt=rt, g_=g_: e.scalar_tensor_tensor(
                        out=rt[:, 44:52], in0=rt[:, 4 + 8 * g_:12 + 8 * g_], scalar=rt[:, 40 + g_:41 + g_], in1=rt[:, 44:52],
                        op0=ALU.mult, op1=ALU.add))
                R("dve", lambda e, rt=rt: e.max(out=rt[:, 52:60], in_=rt[:, 44:52]))
                yield
                R("dve", lambda e, rt=rt: e.tensor_scalar(out=rt[:, 60:61], in0=rt[:, 52:53], scalar1=-1.0, scalar2=None, op0=ALU.mult))
                yield
                R("act", lambda e, rt=rt: e.activation(out=rt[:, 64:72], in_=rt[:, 44:52], func=AF.Exp, bias=rt[:, 60:61]))
                yield
                R("dve", lambda e, rt=rt: e.tensor_scalar(out=rt[:, 72:80], in0=rt[:, 44:52], scalar1=rt[:, 53:54], scalar2=None,
                                                         op0=ALU.is_ge))
                yield
                R("dve", lambda e, rt=rt: e.tensor_tensor(out=rt[:, 80:88], in0=rt[:, 64:72], in1=rt[:, 72:80], op=ALU.mult))
                yield
                R("dve", lambda e, rt=rt: e.tensor_reduce(out=rt[:, 61:62], in_=rt[:, 80:88], axis=AX.X, op=ALU.add))
                yield
                R("dve", lambda e, rt=rt: e.reciprocal(out=rt[:, 62:63], in_=rt[:, 61:62]))
                yield
                R("dve", lambda e, rt=rt: e.tensor_tensor(out=rt[:, 63:64], in0=rt[:, 62:63], in1=rt[:, 39:40], op=ALU.mult))
                yield
                R("dve", lambda e, rt=rt: e.tensor_scalar(out=rt[:, 80:88], in0=rt[:, 80:88], scalar1=rt[:, 63:64], scalar2=None,
                                                         op0=ALU.mult))
                yield
                for g_ in range(4):
                    R("dve", lambda e, rt=rt, g_=g_, tl=tl, cwb=cwb: e.tensor_scalar(
                        out=cwb[:, tl, g_ * 8:(g_ + 1) * 8], in0=rt[:, 80:88], scalar1=rt[:, 40 + g_:41 + g_], scalar2=None,
                        op0=ALU.mult), extra_w=[cwb])
                if dbg and dbg[0] == "cw" and b == 0:
                    _merge(final, P.dma("sp", dbg_d[:, tl * 32:(tl + 1) * 32], cwb[:, tl, :], reads=[cwb]))

                yield

            def moe_block(b):
                units = [(e_, sk) for e_ in range(n_exp) for sk in range(NSUB)]
                W = {}
                if units:
                    W[0] = W_next.pop(0) if 0 in W_next else load_expert(0)
                state = {}

                def s1(u):
                    e_, sk = units[u]
                    if sk == 1 and e_ + 1 == n_exp and b + 1 < n_blk:
                        W_next[0] = load_expert(0)
                    if sk == 1 and e_ + 1 < n_exp:
                        W[e_ + 1] = load_expert(e_ + 1)
                    wg, wu, wd = W[e_]
                    gu = gu_r.next()
                    k = 0
                    for gi, w_ in ((0, wg), (1, wu)):
                        for fc in range(2):
                            for c in range(8):
                                k += 1
                                P.op("pe", lambda e, gu=gu, w_=w_, gi=gi, fc=fc, c=c, sk=sk: e.matmul(
                                    gu[:, (gi * 2 + fc) * 256:(gi * 2 + fc + 1) * 256], lhsT=w_[:, c, fc * 128:(fc + 1) * 128],
                                    rhs=mTb[:, c, sk * 256:(sk + 1) * 256], start=(c == 0), stop=(c == 7)),
                                    reads=[w_, mTb], writes=[gu], partial=True, signal=(k == 32))
                    state[u] = gu

                def s2(u):
                    gu = state[u]
                    sgt, hid = sgt_r.next(), hid_r.next()
                    P.op("act", lambda e: e.activation(out=sgt[:], in_=gu[:, 0:512], func=AF.Silu), reads=[gu], writes=[sgt])
                    P.op("dve", lambda e: e.tensor_tensor(out=hid[:], in0=sgt[:], in1=gu[:, 512:1024], op=ALU.mult),
                         reads=[sgt, gu], writes=[hid])
                    state[u] = hid

                def s3(u):
                    e_, sk = units[u]
                    hid = state.pop(u)
                    wd = W[e_][2]
                    for tt in range(2):
                        tl = sk * 2 + tt
                        y = y_r.next()
                        for half in range(2):
                            for fc in range(2):
                                P.op("pe", lambda e, y=y, half=half, fc=fc, tt=tt: e.matmul(
                                    y[:, half * 512:(half + 1) * 512], lhsT=hid[:, fc * 256 + tt * 128:fc * 256 + (tt + 1) * 128],
                                    rhs=wd[:, fc, half * 512:(half + 1) * 512], start=(fc == 0), stop=(fc == 1)),
                                    reads=[hid, wd], writes=[y], partial=True, signal=(half == 1 and fc == 1))
                        ya = yacc[tl]
                        cwb_ = cw[b]
                        P.op("dve", lambda e, y=y, ya=ya, tl=tl, e_=e_, cwb_=cwb_: e.scalar_tensor_tensor(
                            out=ya[:], in0=y[:], scalar=cwb_[:, tl, e_:e_ + 1], in1=ya[:], op0=ALU.mult, op1=ALU.add),
                            reads=[y, cwb_], writes=[ya])

                if units:
                    s1(0)
                for u in range(len(units)):
                    s2(u)
                    if u + 1 < len(units):
                        s1(u + 1)
                    s3(u)


            def p7_tile(b, tl, sl):
                t = b * TPB + tl
                rows = slice(t * 128, (t + 1) * 128)
                h2 = yacc[tl]
                rp = rstd_of(h2[:], [h2], D)
                yield
                mp = sl["f32"].next()
                P.op("dve", lambda e, h2=h2, rp=rp, mp=mp: e.scalar_tensor_tensor(
                    out=mp[:], in0=h2[:], scalar=rp[:, 0:1], in1=gple[:], op0=ALU.mult, op1=ALU.mult),
                    reads=[h2, rp, gple], writes=[mp])
                yield
                pa = sl["ps"]
                transpose8(pa, mp)
                yield
                mpT = sl["bfT"]
                P.op("act", lambda e, pa=pa, mpT=mpT: e.activation(
                    out=mpT[:], in_=pa[:].rearrange("p (c t) -> p c t", c=8), func=AF.Copy), reads=[pa], writes=[mpT])
                yield
                pg = sl["ps"]
                mm16(pg, mpT, wpg)
                yield
                gate = sl["f32"].next()
                P.op("act", lambda e, pg=pg, gate=gate: e.activation(out=gate[:], in_=pg[:], func=AF.Sigmoid), reads=[pg], writes=[gate])
                yield
                pt_ = sl["p"]
                P.dma("sp", pt_[:], p_d[rows, :], writes=[pt_])
                yield
                pp = sl["ps"]
                transpose8(pp, pt_, n=2)
                yield
                pT = sl["pT"]
                P.op("dve", lambda e, pp=pp, pT=pT: e.tensor_copy(
                    out=pT[:], in_=pp[:, 0:256].rearrange("p (c t) -> p c t", c=2)), reads=[pp], writes=[pT])
                yield
                pq = sl["ps"]
                mm16(pq, pT, wpp, nck=2)
                yield
                tmp = sl["f32"].next()
                P.op("dve", lambda e, gate=gate, pq=pq, tmp=tmp: e.tensor_tensor(out=tmp[:], in0=gate[:], in1=pq[:], op=ALU.mult),
                     reads=[gate, pq], writes=[tmp])
                yield
                h3 = sl["f32"].next()
                P.op("dve", lambda e, tmp=tmp, h2=h2, h3=h3: e.tensor_tensor(out=h3[:], in0=tmp[:], in1=h2[:], op=ALU.add),
                     reads=[tmp, h2], writes=[h3])
                yield
                rf = rstd_of(h3[:], [h3], D)
                yield
                ob = sl["f32"].next()
                P.op("dve", lambda e, h3=h3, rf=rf, ob=ob: e.scalar_tensor_tensor(
                    out=ob[:], in0=h3[:], scalar=rf[:, 0:1], in1=gfin[:], op0=ALU.mult, op1=ALU.mult),
                    reads=[h3, rf, gfin], writes=[ob])
                yield
                _merge(final, P.dma("sp", out_d[rows, :], ob[:], reads=[ob]))
                yield
                yield

            def run_window(jobs):
                jobs = list(jobs)
                active = {}
                while jobs or active:
                    for s_ in range(NSLOT):
                        if s_ not in active and jobs:
                            fn, b_, tl_ = jobs.pop(0)
                            active[s_] = fn(b_, tl_, SL[s_])
                    for s_ in list(active):
                        try:
                            next(active[s_])
                        except StopIteration:
                            del active[s_]

            run_window([(p5_tile, 0, tl) for tl in range(TPB)])
            for b in range(n_blk):
                moe_block(b)
                nxt = [(p7_tile, b, tl) for tl in range(TPB)]
                if b + 1 < n_blk:
                    nxt += [(p5_tile, b + 1, tl) for tl in range(TPB)]
                run_window(nxt)
        P.emit(final)
    return nc


def _consts():
    inv_freq = (1.0 / (np.float32(10000.0) ** (np.arange(0, 32, 2, dtype=np.float32) / np.float32(32)))).astype(np.float32)
    ang = (np.arange(S, dtype=np.float32)[:, None] * inv_freq[None, :]).astype(np.float32)
    cos = np.cos(ang).astype(np.float32)
    sin = np.sin(ang).astype(np.float32)
    p = np.arange(128)[:, None]
    f = np.arange(128)[None, :]
    bias = np.zeros((128, 12, 3, 128), np.float32)
    for k in range(12):
        sl = 2.0 ** (3 - k)
        for off in range(3):
            rel = 128 * (off - 1) + p - f
            bias[:, k, off, :] = np.where(np.abs(rel) <= 64, np.exp(-sl * np.abs(rel).astype(np.float64)), 0.0)
    return cos, sin, bias.reshape(128, 12 * 384)


def _in_maps(inputs):
    g = lambda k: np.ascontiguousarray(np.asarray(inputs[k], dtype=np.float32))
    cos, sin, bias = _consts()
    w_r1 = g("w_r1")[0]
    w_r2 = g("w_r2")[0]
    wr = np.ascontiguousarray(np.concatenate([w_r1, w_r2.transpose(1, 0, 2).reshape(D, 32)], axis=1))
    br = np.ascontiguousarray(np.concatenate([g("b_r1")[0], g("b_r2")[0].reshape(32)]))
    tm = lambda a: np.ascontiguousarray(a.reshape(NT, 128, 16).transpose(1, 0, 2).reshape(128, NT * 16))
    tm2 = lambda a: np.ascontiguousarray(np.repeat(a.reshape(NT, 128, 1, 16), 2, axis=2).transpose(1, 0, 2, 3).reshape(128, NT * 32))
    shared = {
        "w_in": g("w_in")[0], "w_uq": g("w_uq")[0], "w_ukv": g("w_ukv")[0], "w_o": g("w_o")[0],
        "wr": wr, "br": br, "w_e_gate": g("w_e_gate")[0], "w_e_up": g("w_e_up")[0],
        "w_e_down": g("w_e_down")[0], "w_ple_gate": g("w_ple_gate")[0], "w_ple_proj": g("w_ple_proj")[0],
        "g_mix": g("g_mix")[0], "g_cq": g("g_cq")[0], "g_ckv": g("g_ckv")[0],
        "g_out": np.ascontiguousarray(np.concatenate([g("g_out_a")[0], g("g_out_b")[0]])),
        "g_ffn": g("g_ffn")[0], "g_ple": g("g_ple")[0], "g_final": g("g_final"),
        "cos_tm": tm(cos), "sin_tm": tm(sin), "cos2_tm": tm2(cos), "sin2_tm": tm2(sin), "bias_a": bias,
    }
    x = g("x")
    p = g("p")[0]
    maps = []
    for b in range(8):
        m = dict(shared)
        m["x"] = x[b]
        m["p"] = p[b]
        maps.append(m)
    return maps


_NC_CACHE = {}


def kernel(**inputs):
    if "nc" not in _NC_CACHE:
        _NC_CACHE["nc"] = build_program()
    nc = _NC_CACHE["nc"]
    res = run_bass_kernel_spmd(nc, _in_maps(inputs), core_ids=list(range(8)))
    return np.stack([np.asarray(r["out"], dtype=np.float32) for r in res.results], axis=0)
```

```python
import numpy as np
from contextlib import ExitStack
import concourse.bass as bass
import concourse.mybir as mybir
from concourse.bass_utils import run_bass_kernel_spmd

F32 = mybir.dt.float32
BF16 = mybir.dt.bfloat16
ALU = mybir.AluOpType
AF = mybir.ActivationFunctionType
AX = mybir.AxisListType

S = 4096
D = 1024
NT = S // 128
EPS = 1e-6
NE = 32
BLK = 1024
NBLK = S // BLK
TPB = BLK // 128
SUB = 256
NSUB = BLK // SUB
DILS = (1, 4, 16)
import os
K_HPS = [int(c) for c in os.environ.get('K_HPS', '0123')]
K_BRS = [int(c) for c in os.environ.get('K_BRS', '012')]
MLA_SCALE = float(96 ** -0.5)


def _merge(d, t):
    for k, v in t.items():
        if d.get(k, 0) < v:
            d[k] = v


class Buf:
    def __init__(self, t, psum=False):
        self.t = t
        self.w = {}
        self.r = {}
        self.psum = psum

    def __getitem__(self, k):
        return self.t[k]


class Prog:
    ENG = ("pe", "act", "dve", "pool", "sp")

    def __init__(self, nc, es, n_dma_sems=32):
        self.nc = nc
        self.es = es
        self.ops = {k: [] for k in self.ENG}
        self.cnt = {k: 0 for k in self.ENG}
        self.waited = {k: {} for k in self.ENG}
        self.nsem = 0
        self.sem = {k: self.new_sem(k) for k in self.ENG}
        half = n_dma_sems // 2
        self.dsem = [self.new_sem("d") for _ in range(n_dma_sems)]
        self.dcnt = [0] * n_dma_sems
        self.dlast = [None] * n_dma_sems
        self.dring = {"sp": list(range(0, half)), "pool": list(range(half, n_dma_sems))}
        self.dpos = {"sp": 0, "pool": 0}
        self.pending = {k: False for k in self.ENG}

    def new_sem(self, name):
        self.nsem += 1
        return self.es.enter_context(self.nc.semaphore(f"s{name}{self.nsem}"))

    def _wait(self, eng, deps):
        for sem, val in deps.items():
            if sem is self.sem[eng] and val > self.cnt[eng]:
                assert eng == "pe"
                continue
            if self.waited[eng].get(sem, 0) < val:
                self.waited[eng][sem] = val
                self.ops[eng].append(lambda e, sem=sem, val=val: e.wait_ge(sem, val))

    def _deps(self, reads, writes, deps):
        d = {}
        for b in reads:
            _merge(d, b.w)
            if b.psum:
                _merge(d, b.r)
        for b in writes:
            _merge(d, b.w)
            _merge(d, b.r)
        if deps:
            _merge(d, deps)
        return d

    def _register(self, t, reads, writes, partial):
        for b in reads:
            _merge(b.r, t)
        for b in writes:
            if partial:
                _merge(b.w, t)
            else:
                b.w.clear()
                b.w.update(t)
            b.r.clear()

    def op(self, eng, fn, reads=(), writes=(), deps=None, partial=False, signal=True):
        self._wait(eng, self._deps(reads, writes, deps))
        if signal:
            self.cnt[eng] += 1
            sem, val = self.sem[eng], self.cnt[eng]
            self.ops[eng].append(lambda e: fn(e).then_inc(sem, 1))
            self.pending[eng] = False
            t = {sem: val}
            if self.cnt[eng] >= 30000:
                self.sem[eng] = self.new_sem(eng)
                self.cnt[eng] = 0
        else:
            self.ops[eng].append(lambda e: fn(e))
            self.pending[eng] = True
            t = {self.sem[eng]: self.cnt[eng] + 1}
        self._register(t, reads, writes, partial)
        return t

    def dma(self, q, out, in_, reads=(), writes=(), deps=None, partial=False, **kw):
        d = self._deps(reads, writes, deps)
        ring = self.dring[q]
        i = ring[self.dpos[q]]
        self.dpos[q] = (self.dpos[q] + 1) % len(ring)
        if self.dlast[i]:
            _merge(d, self.dlast[i])
        self._wait(q, d)
        self.dcnt[i] += 16
        sem, val = self.dsem[i], self.dcnt[i]
        self.ops[q].append(lambda e: e.dma_start(out=out, in_=in_, **kw).then_inc(sem, 16))
        t = {sem: val}
        self.dlast[i] = t
        self._register(t, reads, writes, partial)
        return t

    def emit(self, final_deps):
        nc = self.nc
        for k in self.ENG:
            if self.pending[k]:
                raise RuntimeError(f"engine {k} ends with unsignaled op")
        self._wait("sp", final_deps)
        with nc.Block() as block:
            @block.tensor
            def _(e):
                for f in self.ops["pe"]:
                    f(e)

            @block.scalar
            def _(e):
                for f in self.ops["act"]:
                    f(e)

            @block.vector
            def _(e):
                for f in self.ops["dve"]:
                    f(e)

            @block.gpsimd
            def _(e):
                for f in self.ops["pool"]:
                    f(e)

            @block.sync
            def _(e):
                for f in self.ops["sp"]:
                    f(e)


class Ring:
    def __init__(self, bufs):
        self.bufs = bufs
        self.i = 0

    def next(self):
        b = self.bufs[self.i]
        self.i = (self.i + 1) % len(self.bufs)
        return b


def build_program(upto="all", dbg=None):
    nc = bass.Bass("TRN2", target_bir_lowering=False)

    def din(name, shape, dt=F32):
        return nc.dram_tensor(name, list(shape), dt, kind="ExternalInput").ap()

    x_d = din("x", [S, D])
    p_d = din("p", [S, 256])
    w_in_d = din("w_in", [D, 2208])
    w_uq_d = din("w_uq", [384, 768])
    w_ukv_d = din("w_ukv", [256, 1024])
    w_o_d = din("w_o", [D, D])
    wr_d = din("wr", [D, 36])
    br_d = din("br", [36])
    weg_d = din("w_e_gate", [NE, D, 256])
    weu_d = din("w_e_up", [NE, D, 256])
    wed_d = din("w_e_down", [NE, 256, D])
    wpg_d = din("w_ple_gate", [D, D])
    wpp_d = din("w_ple_proj", [256, D])
    g_mix_d = din("g_mix", [D])
    g_cq_d = din("g_cq", [384])
    g_ckv_d = din("g_ckv", [256])
    g_out_d = din("g_out", [D])
    g_ffn_d = din("g_ffn", [D])
    g_ple_d = din("g_ple", [D])
    g_fin_d = din("g_final", [D])
    cos_d = din("cos_tm", [128, NT * 16])
    sin_d = din("sin_tm", [128, NT * 16])
    cos2_d = din("cos2_tm", [128, NT * 32])
    sin2_d = din("sin2_tm", [128, NT * 32])
    bias_d = din("bias_a", [128, 12 * 384])
    out_d = nc.dram_tensor("out", [S, D], F32, kind="ExternalOutput").ap()
    o_scr = nc.dram_tensor("o_scr", [S, D], F32, kind="Internal").ap()
    o_scr_b = Buf(None)
    cq_scr = nc.dram_tensor("cq_scr", [3, 128, S], F32, kind="Internal").ap()
    ckv_scr = nc.dram_tensor("ckv_scr", [2, 128, S], F32, kind="Internal").ap()
    kr_scr = nc.dram_tensor("kr_scr", [128, NT * 32], F32, kind="Internal").ap()
    lat_b = Buf(None)
    dbg_d = None
    if dbg is not None:
        dbg_d = nc.dram_tensor("dbg", list(dbg[1]), F32, kind="ExternalOutput").ap()

    def bc(ap1d, n):
        return ap1d.rearrange("(o d) -> o d", o=1).to_broadcast([128, n])

    final = {}
    with ExitStack() as es:
        P = Prog(nc, es)

        def snapshot():
            d = {}
            for k in P.ENG:
                if P.cnt[k] > 0:
                    d[P.sem[k]] = P.cnt[k]
            for i, sm in enumerate(P.dsem):
                if P.dcnt[i] > 0:
                    d[sm] = P.dcnt[i]
            return d

        def sb(name, shape, dt, stack=None):
            b = Buf((stack or es).enter_context(nc.sbuf_tensor(name, list(shape), dt)))
            b.w = snapshot()
            return b

        def ps(name, shape, dt, stack):
            b = Buf(stack.enter_context(nc.psum_tensor(name, list(shape), dt)), psum=True)
            b.w = snapshot()
            return b

        idb = sb("idb", [128, 128], BF16)
        idf = sb("idf", [128, 128], F32)
        for ident in (idb, idf):
            P.op("pool", lambda e, t=ident: e.memset(t[:], 1.0), writes=[ident])
            P.op("pool", lambda e, t=ident: e.affine_select(
                out=t[:], in_=t[:], pattern=[[-1, 128]], compare_op=ALU.is_equal,
                fill=0.0, base=0, channel_multiplier=1), writes=[ident])

        def rstd_from_ss(ss, rstd, n):
            P.op("act", lambda e: e.activation(out=ss[:], in_=ss[:], func=AF.Sqrt, scale=1.0 / n, bias=EPS),
                 writes=[ss])
            P.op("dve", lambda e: e.reciprocal(out=rstd[:], in_=ss[:]), reads=[ss], writes=[rstd])

        def dump(buf_ap, reads, shape_rows):
            t = P.dma("sp", dbg_d, buf_ap, reads=reads)
            _merge(final, t)

        esA = ExitStack()
        eW = ExitStack()
        esL = ExitStack()
        if True:
            aT = sb("aT", [128, 8, S], BF16, esA)
            wqkv = sb("wqkv", [128, 8, 1536], BF16, eW)
            w_in_v = w_in_d.rearrange("(c p) n -> p c n", p=128)
            for c in range(8):
                P.dma("pool", wqkv[:, c, :], w_in_v[:, c, 0:1536], writes=[wqkv], partial=True,
                      max_dma_last_dim=4096)
            biasA = sb("biasA", [128, 12, 384], BF16, eW)
            P.dma("pool", biasA[:].rearrange("p a b -> p (a b)"), bias_d, writes=[biasA],
                  max_dma_last_dim=4096)
            with ExitStack() as e1:
                gmix = sb("gmix", [128, D], F32, e1)
                P.dma("sp", gmix[:], bc(g_mix_d, D), writes=[gmix])
                xr = Ring([sb(f"x{i}", [128, D], F32, e1) for i in range(3)])
                sq = sb("sq", [128, D], F32, e1)
                abr = Ring([sb(f"ab{i}", [128, D], BF16, e1) for i in range(2)])
                ssr = Ring([sb(f"ss{i}", [128, 1], F32, e1) for i in range(2)])
                rsr = Ring([sb(f"rs{i}", [128, 1], F32, e1) for i in range(2)])
                tpr = Ring([ps(f"tp{i}", [128, 8, 128], BF16, e1) for i in range(2)])
                def p1_a(t):
                    xb = xr.next()
                    P.dma("sp", xb[:], x_d[t * 128:(t + 1) * 128, :], writes=[xb])
                    ss, rs, ab = ssr.next(), rsr.next(), abr.next()
                    P.op("act", lambda e: e.activation(out=sq[:], in_=xb[:], func=AF.Square, accum_out=ss[:]),
                         reads=[xb], writes=[sq, ss])
                    rstd_from_ss(ss, rs, D)
                    P.op("dve", lambda e: e.scalar_tensor_tensor(
                        out=ab[:], in0=xb[:], scalar=rs[:, 0:1], in1=gmix[:], op0=ALU.mult, op1=ALU.mult),
                        reads=[xb, rs, gmix], writes=[ab])
                    return ab

                def p1_b(t, ab):
                    tp = tpr.next()
                    for c in range(8):
                        P.op("pe", lambda e, c=c: e.transpose(tp[:, c, :], ab[:, c * 128:(c + 1) * 128], idb[:]),
                             reads=[ab, idb], writes=[tp], partial=True, signal=(c == 7))
                    P.op("act", lambda e: e.activation(out=aT[:, :, t * 128:(t + 1) * 128], in_=tp[:], func=AF.Copy),
                         reads=[tp], writes=[aT], partial=True)

                prev_ab = p1_a(0)
                for t in range(NT):
                    nxt_ab = p1_a(t + 1) if t + 1 < NT else None
                    p1_b(t, prev_ab)
                    prev_ab = nxt_ab
            if dbg and dbg[0] == "aT":
                stg = sb("dstg", [128, 8 * 256], F32, esA)
                P.op("dve", lambda e: e.tensor_copy(out=stg[:].rearrange("p (c t) -> p c t", c=8),
                                                    in_=aT[:, :, 0:256]), reads=[aT], writes=[stg])
                dump(stg[:], [stg], None)
            if upto == "p1":
                P.emit(final)
                return nc

            with ExitStack() as e3:
                QT = sb("QT", [128, S], BF16, e3)
                KT = sb("KT", [128, S], BF16, e3)
                QTd = {1: QT, 4: sb("QT4", [128, 4, S // 4], BF16, e3), 16: sb("QT16", [128, 16, S // 16], BF16, e3)}
                KTd = {1: KT, 4: sb("KT4", [128, 4, S // 4], BF16, e3), 16: sb("KT16", [128, 16, S // 16], BF16, e3)}
                Vp = sb("Vp", [128, NT, 2, 66], BF16, e3)
                P.op("pool", lambda e: e.memset(Vp[:], 1.0), writes=[Vp])
                Oacc = [sb(f"Oacc{i}", [65, S], F32, e3) for i in range(2)]
                oa_st = sb("oa_st", [128, NT, 128], F32, e3)
                PTr = Ring([sb(f"PT{i}", [128, 3, 128], BF16, e3) for i in range(3)])
                PRr = Ring([sb(f"PR{i}", [128, 3, 128], BF16, e3) for i in range(2)])
                rcr = Ring([sb(f"rc{i}", [128, 4, 1], F32, e3) for i in range(2)])
                pj = Ring([ps(f"pj{i}", [128, 512], F32, e3) for i in range(2)])
                scr_ = Ring([ps(f"sc{i}", [128, 3, 128], F32, e3) for i in range(2)])
                opr = Ring([ps(f"op{i}", [65, 128], F32, e3) for i in range(2)])
                otr = Ring([ps(f"ot{i}", [128, 4, 65], F32, e3) for i in range(2)])
                for hp in K_HPS:
                    for (dst, col0) in ((QT, hp * 128), (KT, 512 + hp * 128)):
                        for blk in range(8):
                            pp = pj.next()
                            for c in range(8):
                                P.op("pe", lambda e, pp=pp, c=c, col0=col0, blk=blk: e.matmul(
                                    pp[:], lhsT=wqkv[:, c, col0:col0 + 128],
                                    rhs=aT[:, c, blk * 512:(blk + 1) * 512], start=(c == 0), stop=(c == 7)),
                                    reads=[wqkv, aT], writes=[pp], partial=True, signal=(c == 7))
                            if dst is QT:
                                P.op("act", lambda e, pp=pp, blk=blk: e.activation(
                                    out=QT[:, blk * 512:(blk + 1) * 512], in_=pp[:], func=AF.Copy, scale=0.125),
                                    reads=[pp], writes=[QT], partial=True)
                                for d_ in (4, 16):
                                    P.op("act", lambda e, pp=pp, blk=blk, d_=d_: e.activation(
                                        out=QTd[d_][:, :, blk * 512 // d_:(blk + 1) * 512 // d_],
                                        in_=pp[:].rearrange("p (l r) -> p r l", r=d_), func=AF.Copy, scale=0.125),
                                        reads=[pp], writes=[QTd[d_]], partial=True)
                            else:
                                P.op("act", lambda e, pp=pp, blk=blk: e.activation(
                                    out=KT[:, blk * 512:(blk + 1) * 512], in_=pp[:], func=AF.Copy),
                                    reads=[pp], writes=[KT], partial=True)
                                for d_ in (4, 16):
                                    P.op("act", lambda e, pp=pp, blk=blk, d_=d_: e.activation(
                                        out=KTd[d_][:, :, blk * 512 // d_:(blk + 1) * 512 // d_],
                                        in_=pp[:].rearrange("p (l r) -> p r l", r=d_), func=AF.Copy),
                                        reads=[pp], writes=[KTd[d_]], partial=True)
                    for bi, dil in enumerate(DILS):
                        if bi not in K_BRS:
                            continue
                        L = S // dil
                        ntl = L // 128
                        for r in range(dil):
                            for j in range(ntl):
                                ch = r * ntl + j
                                t0 = j * 128 * dil + r
                                pp = pj.next()
                                for c in range(8):
                                    P.op("pe", lambda e, pp=pp, c=c, t0=t0, dil=dil, hp=hp: e.matmul(
                                        pp[:, 0:128], lhsT=aT[:, c, t0:t0 + 127 * dil + 1:dil],
                                        rhs=wqkv[:, c, 1024 + hp * 128:1024 + (hp + 1) * 128],
                                        start=(c == 0), stop=(c == 7)),
                                        reads=[wqkv, aT], writes=[pp], partial=True, signal=(c == 7))
                                P.op("act", lambda e, pp=pp, ch=ch: e.activation(
                                    out=Vp[:, ch, :, 0:64], in_=pp[:, 0:128].rearrange("p (h d) -> p h d", h=2),
                                    func=AF.Copy), reads=[pp], writes=[Vp], partial=True)
                        its = [(hh, r, i) for hh in range(2) for r in range(dil) for i in range(ntl)]

                        def a_score(it, bi=bi, dil=dil, ntl=ntl, hp=hp):
                            hh, r, i = it
                            h = hp * 2 + hh
                            bt = 3 - (-(h + 1) + 2 * bi)
                            pr0 = hh * 64
                            js = [j for j in (i - 1, i, i + 1) if 0 <= j < ntl]
                            sc = scr_.next()
                            q0 = i * 128 * dil + r
                            for n_, j in enumerate(js):
                                off = j - i + 1
                                k0 = j * 128 * dil + r
                                if dil == 1:
                                    lT_ = KT[pr0:pr0 + 64, j * 128:(j + 1) * 128]
                                    rh_ = QT[pr0:pr0 + 64, i * 128:(i + 1) * 128]
                                else:
                                    lT_ = KTd[dil][pr0:pr0 + 64, r, j * 128:(j + 1) * 128]
                                    rh_ = QTd[dil][pr0:pr0 + 64, r, i * 128:(i + 1) * 128]
                                P.op("pe", lambda e, sc=sc, off=off, lT_=lT_, rh_=rh_: e.matmul(
                                    sc[:, off, :], lhsT=lT_, rhs=rh_, start=True, stop=True),
                                    reads=[KTd[dil], QTd[dil]], writes=[sc], partial=True, signal=(n_ == len(js) - 1))
                            return (sc, js, q0, bt)

                        def a_exp(it, st_, nxt=None):
                            hh, r, i = it
                            sc, js, q0, bt = st_
                            o0 = js[0] - i + 1
                            o1 = js[-1] - i + 2
                            pr_, pt = PRr.next(), PTr.next()
                            P.op("act", lambda e: e.activation(out=pr_[:, o0:o1, :], in_=sc[:, o0:o1, :], func=AF.Exp),
                                 reads=[sc], writes=[pr_])
                            nst = a_score(nxt) if nxt is not None else None
                            P.op("dve", lambda e: e.tensor_tensor(
                                out=pt[:, o0:o1, :], in0=pr_[:, o0:o1, :],
                                in1=biasA[:, bt, o0 * 128:o1 * 128].rearrange("p (a b) -> p a b", b=128), op=ALU.mult),
                                reads=[pr_, biasA], writes=[pt])
                            return nst, (it, js, q0, pt)

                        def a_pv(it, js, q0, pt, bi=bi, dil=dil, ntl=ntl):
                            hh, r, i = it
                            opp = opr.next()
                            for n_, j in enumerate(js):
                                off = j - i + 1
                                ch = r * ntl + j
                                P.op("pe", lambda e, off=off, ch=ch, n_=n_, nj=len(js): e.matmul(
                                    opp[:], lhsT=Vp[:, ch, hh, 0:65], rhs=pt[:, off, :],
                                    start=(n_ == 0), stop=(n_ == nj - 1)),
                                    reads=[Vp, pt], writes=[opp], partial=True, signal=(n_ == len(js) - 1))
                            oa = Oacc[hh]
                            if bi == 0:
                                P.op("dve", lambda e: e.tensor_copy(out=oa[:, q0:q0 + 127 * dil + 1:dil], in_=opp[:]),
                                     reads=[opp], writes=[oa], partial=True)
                            else:
                                P.op("dve", lambda e: e.tensor_tensor(
                                    out=oa[:, q0:q0 + 127 * dil + 1:dil], in0=oa[:, q0:q0 + 127 * dil + 1:dil],
                                    in1=opp[:], op=ALU.add), reads=[opp], writes=[oa], partial=True)

                        st_ = a_score(its[0])
                        pv_pend = None
                        for n_it, it in enumerate(its):
                            st_, pv_new = a_exp(it, st_, nxt=(its[n_it + 1] if n_it + 1 < len(its) else None))
                            if pv_pend is not None:
                                a_pv(*pv_pend)
                            pv_pend = pv_new
                        a_pv(*pv_pend)
                    for hh in range(2):
                        oa = Oacc[hh]
                        for t4 in range(NT // 4):
                            ot = otr.next()
                            rc = rcr.next()
                            for k4 in range(4):
                                t = t4 * 4 + k4
                                P.op("pe", lambda e, ot=ot, oa=oa, t=t, k4=k4: e.transpose(
                                    ot[:, k4, :], oa[:, t * 128:(t + 1) * 128], idf[0:65, 0:65]),
                                    reads=[oa, idf], writes=[ot], partial=True, signal=(k4 == 3))
                            P.op("dve", lambda e, ot=ot, rc=rc: e.reciprocal(out=rc[:], in_=ot[:, :, 64:65]),
                                 reads=[ot], writes=[rc])
                            for k4 in range(4):
                                t = t4 * 4 + k4
                                P.op("dve", lambda e, ot=ot, rc=rc, t=t, hh=hh, k4=k4: e.tensor_scalar(
                                    out=oa_st[:, t, hh * 64:(hh + 1) * 64], in0=ot[:, k4, 0:64], scalar1=rc[:, k4, 0:1],
                                    scalar2=None, op0=ALU.mult), reads=[ot, rc], writes=[oa_st], partial=True)
                    for q4 in range(4):
                        t = P.dma("sp", o_scr.rearrange("(t p) d -> p t d", p=128)[:, q4 * 8:(q4 + 1) * 8,
                                                                                hp * 128:(hp + 1) * 128],
                                  oa_st[:, q4 * 8:(q4 + 1) * 8, :], reads=[oa_st], writes=[o_scr_b], partial=True)
                        _merge(final, t)
                if dbg and dbg[0] == "wdump":
                    _merge(final, P.dma("pool", dbg_d[:, 0:1536], wqkv[:, 0, :], reads=[wqkv]))
                    _merge(final, P.dma("pool", dbg_d[:, 1536:3072], wqkv[:, 7, :], reads=[wqkv]))
                    _merge(final, P.dma("pool", dbg_d[:, 3072:3072 + 384], biasA[:, 11, :], reads=[biasA]))
                if dbg and dbg[0] == "p3dump":
                    _merge(final, P.dma("pool", dbg_d[:, 0:512], QT[:, 0:512], reads=[QT]))
                    _merge(final, P.dma("pool", dbg_d[:, 512:1024], KT[:, 0:512], reads=[KT]))
                    _merge(final, P.dma("pool", dbg_d[:, 1024:3136], Vp[:, 0:16, :, :].rearrange("p a b c -> p (a b c)"), reads=[Vp]))
                    _merge(final, P.dma("sp", dbg_d[0:65, 3136:3648], Oacc[0][:, 0:512], reads=[Oacc[0]]))
            if dbg and dbg[0] == "o_scr_a":
                _merge(final, P.dma("sp", dbg_d, o_scr[:, 0:512], reads=[o_scr_b]))
            if upto == "p3":
                P.emit(final)
                return nc

            eW.close()
            cqT = sb("cqT", [128, 3, S], BF16, esL)
            ckvT = sb("ckvT", [128, 2, S], BF16, esL)
            krt = sb("krt", [128, NT, 32], BF16, esL)
            with ExitStack() as e2:
                wlat = sb("wlat", [128, 8, 672], BF16, e2)
                for c in range(8):
                    P.dma("pool", wlat[:, c, :], w_in_v[:, c, 1536:2208], writes=[wlat], partial=True)
                gcq = sb("gcq", [128, 384], F32, e2)
                gckv = sb("gckv", [128, 256], F32, e2)
                P.dma("sp", gcq[:], bc(g_cq_d, 384), writes=[gcq])
                P.dma("sp", gckv[:], bc(g_ckv_d, 256), writes=[gckv])
                cosb = sb("cosb", [128, NT, 16], F32, e2)
                sinb = sb("sinb", [128, NT, 16], F32, e2)
                P.dma("sp", cosb[:].rearrange("p t f -> p (t f)"), cos_d, writes=[cosb])
                P.dma("sp", sinb[:].rearrange("p t f -> p (t f)"), sin_d, writes=[sinb])
                LAr = Ring([ps(f"LA{i}", [128, 384], F32, e2) for i in range(2)])
                LBr = Ring([ps(f"LB{i}", [128, 288], F32, e2) for i in range(2)])
                tqr = Ring([ps(f"tq{i}", [128, 5, 128], BF16, e2) for i in range(2)])
                sq2 = sb("sq2", [128, 384], F32, e2)
                cqr = Ring([sb(f"cqn{i}", [128, 640], BF16, e2) for i in range(2)])
                tmr = Ring([sb(f"tm{i}", [128, 64], F32, e2) for i in range(2)])
                s4r = Ring([sb(f"s4{i}", [128, 4], F32, e2) for i in range(2)])
                def p2_a(t):
                    LA, LB, tq, cqn, tm, s4 = LAr.next(), LBr.next(), tqr.next(), cqr.next(), tmr.next(), s4r.next()
                    tk = slice(t * 128, (t + 1) * 128)
                    for (dst, c0, c1) in ((LA, 0, 384), (LB, 384, 672)):
                        for c in range(8):
                            P.op("pe", lambda e, dst=dst, c=c, c0=c0, c1=c1, tk=tk: e.matmul(
                                dst[:], lhsT=aT[:, c, tk], rhs=wlat[:, c, c0:c1], start=(c == 0), stop=(c == 7)),
                                reads=[aT, wlat], writes=[dst], partial=True, signal=(c == 7))
                    ssq, sskv, rq, rkv = (Buf(s4.t[:, i:i + 1]) for i in range(4))
                    for b_ in (ssq, sskv, rq, rkv):
                        b_.w = dict(s4.w); b_.r = dict(s4.r)
                    P.op("act", lambda e, LA=LA, ssq=ssq: e.activation(out=sq2[:, 0:384], in_=LA[:], func=AF.Square,
                                                                      accum_out=ssq.t), reads=[LA], writes=[sq2, ssq])
                    P.op("act", lambda e, LB=LB, sskv=sskv: e.activation(out=sq2[:, 0:256], in_=LB[:, 0:256], func=AF.Square,
                                                                        accum_out=sskv.t), reads=[LB], writes=[sq2, sskv])
                    for (ss_, rs_, n_) in ((ssq, rq, 384), (sskv, rkv, 256)):
                        P.op("act", lambda e, ss_=ss_, n_=n_: e.activation(out=ss_.t, in_=ss_.t, func=AF.Sqrt,
                                                                          scale=1.0 / n_, bias=EPS), writes=[ss_])
                        P.op("dve", lambda e, ss_=ss_, rs_=rs_: e.reciprocal(out=rs_.t, in_=ss_.t), reads=[ss_], writes=[rs_])
                    P.op("dve", lambda e, LA=LA, rq=rq, cqn=cqn: e.scalar_tensor_tensor(
                        out=cqn[:, 0:384], in0=LA[:], scalar=rq.t, in1=gcq[:], op0=ALU.mult, op1=ALU.mult),
                        reads=[LA, rq, gcq], writes=[cqn], partial=True)
                    P.op("dve", lambda e, LB=LB, rkv=rkv, cqn=cqn: e.scalar_tensor_tensor(
                        out=cqn[:, 384:640], in0=LB[:, 0:256], scalar=rkv.t, in1=gckv[:], op0=ALU.mult, op1=ALU.mult),
                        reads=[LB, rkv, gckv], writes=[cqn], partial=True)
                    _merge(s4.r, rq.r); _merge(s4.r, rkv.r); _merge(s4.r, ssq.r); _merge(s4.r, sskv.r)
                    _merge(s4.w, rq.w); _merge(s4.w, rkv.w); _merge(s4.w, ssq.w); _merge(s4.w, sskv.w)
                    for (k_, xa, tb) in ((0, 256, cosb), (1, 272, sinb), (2, 256, sinb), (3, 272, cosb)):
                        P.op("dve", lambda e, LB=LB, tm=tm, k_=k_, xa=xa, tb=tb, t=t: e.tensor_tensor(
                            out=tm[:, k_ * 16:(k_ + 1) * 16], in0=LB[:, xa:xa + 16], in1=tb[:, t, :], op=ALU.mult),
                            reads=[LB, tb], writes=[tm], partial=True)
                    P.op("dve", lambda e, tm=tm, t=t: e.tensor_tensor(out=krt[:, t, 0:16], in0=tm[:, 0:16], in1=tm[:, 16:32],
                                                                     op=ALU.subtract), reads=[tm], writes=[krt], partial=True)
                    P.op("dve", lambda e, tm=tm, t=t: e.tensor_tensor(out=krt[:, t, 16:32], in0=tm[:, 32:48], in1=tm[:, 48:64],
                                                                     op=ALU.add), reads=[tm], writes=[krt], partial=True)
                    return (tq, cqn, tk)

                def p2_b(t, tq, cqn, tk):
                    for c in range(5):
                        P.op("pe", lambda e, tq=tq, cqn=cqn, c=c: e.transpose(tq[:, c, :], cqn[:, c * 128:(c + 1) * 128], idb[:]),
                             reads=[cqn, idb], writes=[tq], partial=True, signal=(c == 4))
                    P.op("act", lambda e, tq=tq, tk=tk: e.activation(out=cqT[:, :, tk], in_=tq[:, 0:3, :], func=AF.Copy),
                         reads=[tq], writes=[cqT], partial=True)
                    P.op("act", lambda e, tq=tq, tk=tk: e.activation(out=ckvT[:, :, tk], in_=tq[:, 3:5, :], func=AF.Copy),
                         reads=[tq], writes=[ckvT], partial=True)

                prev2 = p2_a(0)
                for t in range(NT):
                    nxt2 = p2_a(t + 1) if t + 1 < NT else None
                    p2_b(t, *prev2)
                    prev2 = nxt2
                if dbg and dbg[0] == "lat":
                    for c in range(3):
                        _merge(final, P.dma("pool", dbg_d[:, c * 4096:(c + 1) * 4096], cqT[:, c, :], reads=[cqT]))
            if upto == "p2":
                P.emit(final)
                return nc

        with ExitStack() as esM:
            wuq = sb("wuq", [128, 3, 768], BF16, esM)
            wukv = sb("wukv", [128, 2, 1024], BF16, esM)
            P.dma("pool", wuq[:], w_uq_d.rearrange("(c p) n -> p c n", p=128), writes=[wuq])
            P.dma("pool", wukv[:], w_ukv_d.rearrange("(c p) n -> p c n", p=128), writes=[wukv])
            cos2 = sb("cos2", [128, NT, 2, 16], F32, esM)
            sin2 = sb("sin2", [128, NT, 2, 16], F32, esM)
            P.dma("sp", cos2[:].rearrange("p t h f -> p (t h f)"), cos2_d, writes=[cos2])
            P.dma("sp", sin2[:].rearrange("p t h f -> p (t h f)"), sin2_d, writes=[sin2])
            QT2 = sb("QT2", [128, 2, S], BF16, esM)
            KT2 = sb("KT2", [128, 2, S], BF16, esM)
            P.op("pool", lambda e: e.memset(QT2[:], 0.0), writes=[QT2])
            P.op("pool", lambda e: e.memset(KT2[:], 0.0), writes=[KT2])
            Vp2 = sb("Vp2", [128, NT, 2, 66], BF16, esM)
            P.op("pool", lambda e: e.memset(Vp2[:], 1.0), writes=[Vp2])
            ob_st = sb("ob_st", [128, NT, 128], F32, esM)
            Gm = ps("Gm", [128, 512], F32, esM)

            def view(ap):
                v = Buf(ap, psum=True)
                v.w, v.r = Gm.w, Gm.r
                return v
            Qp_r = Ring([view(Gm.t[:, 0:192].rearrange("p (h d) -> p h d", h=2))])
            KVp_r = Ring([ps("KVps", [128, 2, 128], F32, esM)])
            ot_r = Ring([view(Gm.t[:, 0:260].rearrange("p (k d) -> p k d", k=4))])
            tQK_r = Ring([ps(f"tQK{i}", [128, 4, 128], BF16, esM) for i in range(1)])
            St_r = Ring([ps(f"St{i}", [128, 1024], F32, esM) for i in range(2)])
            O_r = Ring([ps(f"Ops{i}", [65, 512], F32, esM) for i in range(1)])
            Qtm_r = Ring([sb(f"Qtm{i}", [128, 2, 96], BF16, esM) for i in range(2)])
            Ktm_r = Ring([sb(f"Ktm{i}", [128, 2, 96], BF16, esM) for i in range(2)])
            tm2_r = Ring([sb(f"tm2{i}", [128, 4, 2, 16], F32, esM) for i in range(2)])
            PT2_r = Ring([sb(f"PTb{i}", [128, 1024], BF16, esM) for i in range(3)])
            Osb_r = Ring([sb(f"Osb{i}", [65, 512], F32, esM) for i in range(2)])
            rc2_r = Ring([sb(f"rcb{i}", [128, 4, 1], F32, esM) for i in range(2)])
            def mb_s1(hp, t):
                tk = slice(t * 128, (t + 1) * 128)
                Qps, KVps, Qtm, Ktm, tm2 = Qp_r.next(), KVp_r.next(), Qtm_r.next(), Ktm_r.next(), tm2_r.next()
                for c in range(3):
                    P.op("pe", lambda e, c=c: e.matmul(
                        Qps[:].rearrange("p h d -> p (h d)"), lhsT=cqT[:, c, tk], rhs=wuq[:, c, hp * 192:(hp + 1) * 192],
                        start=(c == 0), stop=(c == 2)), reads=[cqT, wuq], writes=[Qps], partial=True, signal=(c == 2))
                for c in range(2):
                    P.op("pe", lambda e, c=c: e.matmul(
                        KVps[:].rearrange("p h d -> p (h d)"), lhsT=ckvT[:, c, tk], rhs=wukv[:, c, hp * 256:(hp + 1) * 256],
                        start=(c == 0), stop=(c == 1)), reads=[ckvT, wukv], writes=[KVps], partial=True, signal=(c == 1))
                P.op("dve", lambda e: e.tensor_copy(out=Qtm[:, :, 0:64], in_=Qps[:, :, 0:64]),
                     reads=[Qps], writes=[Qtm], partial=True)
                P.op("dve", lambda e: e.tensor_copy(out=Ktm[:, :, 0:64], in_=KVps[:, :, 0:64]),
                     reads=[KVps], writes=[Ktm], partial=True)
                P.op("dve", lambda e: e.tensor_copy(out=Vp2[:, t, :, 0:64], in_=KVps[:, :, 64:128]),
                     reads=[KVps], writes=[Vp2], partial=True)
                for (k_, xa, tb) in ((0, 64, cos2), (1, 80, sin2), (2, 64, sin2), (3, 80, cos2)):
                    P.op("dve", lambda e, k_=k_, xa=xa, tb=tb: e.tensor_tensor(
                        out=tm2[:, k_, :, :], in0=Qps[:, :, xa:xa + 16], in1=tb[:, t, :, :], op=ALU.mult),
                        reads=[Qps, tb], writes=[tm2], partial=True)
                P.op("dve", lambda e: e.tensor_tensor(out=Qtm[:, :, 64:80], in0=tm2[:, 0, :, :], in1=tm2[:, 1, :, :],
                                                     op=ALU.subtract), reads=[tm2], writes=[Qtm], partial=True)
                P.op("dve", lambda e: e.tensor_tensor(out=Qtm[:, :, 80:96], in0=tm2[:, 2, :, :], in1=tm2[:, 3, :, :],
                                                     op=ALU.add), reads=[tm2], writes=[Qtm], partial=True)
                for hh in range(2):
                    P.op("pool", lambda e, hh=hh: e.tensor_copy(out=Ktm[:, hh, 64:96], in_=krt[:, t, :]),
                         reads=[krt], writes=[Ktm], partial=True)
                return (Qtm, Ktm)

            def mb_s2(t, Qtm, Ktm):
                tk = slice(t * 128, (t + 1) * 128)
                tQK = tQK_r.next()
                for hh in range(2):
                    P.op("pe", lambda e, hh=hh: e.transpose(tQK[0:96, hh, :], Qtm[:, hh, :], idb[:]),
                         reads=[Qtm, idb], writes=[tQK], partial=True, signal=False)
                for hh in range(2):
                    P.op("pe", lambda e, hh=hh: e.transpose(tQK[0:96, 2 + hh, :], Ktm[:, hh, :], idb[:]),
                         reads=[Ktm, idb], writes=[tQK], partial=True, signal=(hh == 1))
                P.op("act", lambda e: e.activation(out=QT2[0:96, :, tk], in_=tQK[0:96, 0:2, :], func=AF.Copy),
                     reads=[tQK], writes=[QT2], partial=True)
                P.op("act", lambda e: e.activation(out=KT2[0:96, :, tk], in_=tQK[0:96, 2:4, :], func=AF.Copy),
                     reads=[tQK], writes=[KT2], partial=True)

            for hp in K_HPS:
                prev = mb_s1(hp, 0)
                for t in range(NT):
                    nxt_ = mb_s1(hp, t + 1) if t + 1 < NT else None
                    mb_s2(t, *prev)
                    prev = nxt_
                for hh in range(int(os.environ.get('K_P4H', 2))):
                    for qb in range(int(os.environ.get('K_P4Q', 8))):
                        qs = slice(qb * 512, (qb + 1) * 512)
                        Ops = O_r.next()

                        def b_score(kp, hh=hh, qs=qs):
                            St = St_r.next()
                            for j2 in range(2):
                                kc = 2 * kp + j2
                                P.op("pe", lambda e, St=St, hh=hh, kc=kc, qs=qs, j2=j2: e.matmul(
                                    St[:, j2 * 512:(j2 + 1) * 512], lhsT=KT2[:, hh, kc * 128:(kc + 1) * 128], rhs=QT2[:, hh, qs],
                                    start=True, stop=True),
                                    reads=[KT2, QT2], writes=[St], partial=True, signal=(j2 == 1))
                            return St
                        NP_ = NT // 2

                        def b_pv(kp, PT, hh=hh, Ops=Ops):
                            for j2 in range(2):
                                kc = 2 * kp + j2
                                P.op("pe", lambda e, kc=kc, j2=j2: e.matmul(
                                    Ops[:], lhsT=Vp2[:, kc, hh, 0:65], rhs=PT[:, j2 * 512:(j2 + 1) * 512],
                                    start=(kc == 0), stop=(kc == NT - 1)),
                                    reads=[Vp2, PT], writes=[Ops], partial=True, signal=(kc == NT - 1))
                        St_next = b_score(0)
                        pv_pend = None
                        for kp in range(NP_):
                            St, PT = St_next, PT2_r.next()
                            P.op("act", lambda e, St=St, PT=PT: e.activation(out=PT[:], in_=St[:], func=AF.Exp, scale=MLA_SCALE),
                                 reads=[St], writes=[PT])
                            if kp + 1 < NP_:
                                St_next = b_score(kp + 1)
                            if pv_pend is not None:
                                b_pv(*pv_pend)
                            pv_pend = (kp, PT)
                        b_pv(*pv_pend)
                        Osb = Osb_r.next()
                        P.op("dve", lambda e, Osb=Osb, Ops=Ops: e.tensor_copy(out=Osb[:], in_=Ops[:]), reads=[Ops], writes=[Osb])
                        ot, rc = ot_r.next(), rc2_r.next()
                        for k4 in range(4):
                            P.op("pe", lambda e, ot=ot, Osb=Osb, k4=k4: e.transpose(
                                ot[:, k4, :], Osb[:, k4 * 128:(k4 + 1) * 128], idf[0:65, 0:65]),
                                reads=[Osb, idf], writes=[ot], partial=True, signal=(k4 == 3))
                        P.op("dve", lambda e, ot=ot, rc=rc: e.reciprocal(out=rc[:], in_=ot[:, :, 64:65]), reads=[ot], writes=[rc])
                        for k4 in range(4):
                            t = qb * 4 + k4
                            P.op("dve", lambda e, ot=ot, rc=rc, t=t, hh=hh, k4=k4: e.tensor_scalar(
                                out=ob_st[:, t, hh * 64:(hh + 1) * 64], in0=ot[:, k4, 0:64], scalar1=rc[:, k4, 0:1], scalar2=None,
                                op0=ALU.mult), reads=[ot, rc], writes=[ob_st], partial=True)
                for q4 in range(4):
                    t_ = P.dma("sp", o_scr.rearrange("(t p) d -> p t d", p=128)[:, q4 * 8:(q4 + 1) * 8,
                                                                             512 + hp * 128:512 + (hp + 1) * 128],
                               ob_st[:, q4 * 8:(q4 + 1) * 8, :], reads=[ob_st], writes=[o_scr_b], partial=True)
                    _merge(final, t_)
            if dbg and dbg[0] == "p4in":
                _merge(final, P.dma("pool", dbg_d[:, 0:4096], cqT[:, 2, :], reads=[cqT]))
                _merge(final, P.dma("pool", dbg_d[:, 4096:4864], wuq[:, 2, :], reads=[wuq]))
                _merge(final, P.dma("pool", dbg_d[:, 4864:5888], wukv[:, 1, :], reads=[wukv]))
                _merge(final, P.dma("pool", dbg_d[:, 5888:6912], krt[:].rearrange("p t f -> p (t f)"), reads=[krt]))
            if dbg and dbg[0] == "p4dump":
                _merge(final, P.dma("pool", dbg_d[:, 0:512], QT2[:, 0, 0:512], reads=[QT2]))
                _merge(final, P.dma("pool", dbg_d[:, 512:1024], KT2[:, 0, 0:512], reads=[KT2]))
                _merge(final, P.dma("pool", dbg_d[:, 1024:3136], Vp2[:, 0:16, :, :].rearrange("p a b c -> p (a b c)"), reads=[Vp2]))
                _merge(final, P.dma("sp", dbg_d[0:65, 3136:3648], Osb_r.bufs[0][:, :], reads=[Osb_r.bufs[0]]))
            if dbg and dbg[0] == "o_scr":
                _merge(final, P.dma("sp", dbg_d, o_scr, reads=[o_scr_b]))
        esL.close()
        esA.close()
        if upto == "p4":
            P.emit(final)
            return nc

        with ExitStack() as esF:
            wo = sb("wo", [128, 8, D], BF16, esF)
            wpg = sb("wpg", [128, 8, D], BF16, esF)
            wpp = sb("wpp", [128, 2, D], BF16, esF)
            for c in range(8):
                P.dma("pool", wo[:, c, :], w_o_d[c * 128:(c + 1) * 128, :], writes=[wo], partial=True, max_dma_last_dim=4096)
            wrf = sb("wrf", [128, 8, 36], F32, esF)
            P.dma("sp", wrf[:], wr_d.rearrange("(c p) n -> p c n", p=128), writes=[wrf])
            wrh = sb("wrh", [128, 8, 36], BF16, esF)
            wrl = sb("wrl", [128, 8, 36], BF16, esF)
            P.op("dve", lambda e: e.tensor_copy(out=wrh[:], in_=wrf[:]), reads=[wrf], writes=[wrh])
            P.op("dve", lambda e: e.tensor_tensor(out=wrl[:], in0=wrf[:], in1=wrh[:], op=ALU.subtract), reads=[wrf, wrh], writes=[wrl])
            gains = {}
            for nm, gd in (("gout", g_out_d), ("gffn", g_ffn_d), ("gple", g_ple_d), ("gfin", g_fin_d)):
                gains[nm] = sb(nm, [128, D], F32, esF)
                P.dma("sp", gains[nm][:], bc(gd, D), writes=[gains[nm]])
            gout, gffn, gple, gfin = gains["gout"], gains["gffn"], gains["gple"], gains["gfin"]
            brb = sb("brb", [128, 36], F32, esF)
            P.dma("sp", brb[:], bc(br_d, 36), writes=[brb])
            for c in range(8):
                P.dma("pool", wpg[:, c, :], wpg_d[c * 128:(c + 1) * 128, :], writes=[wpg], partial=True, max_dma_last_dim=4096)
            for c in range(2):
                P.dma("pool", wpp[:, c, :], wpp_d[c * 128:(c + 1) * 128, :], writes=[wpp], partial=True, max_dma_last_dim=4096)
            yacc = [sb(f"yacc{i}", [128, D], F32, esF) for i in range(TPB)]
            mTb = sb("mTb", [128, 8, BLK], BF16, esF)
            cw_one = sb("cw", [128, TPB, 32], F32, esF)
            cw = [cw_one] * NBLK
            PS = [ps(f"PS{i}", [128, 1024], F32, esF) for i in range(4)]
            psr = Ring(PS)
            gu_r = Ring(PS[0:2])
            y_r = Ring(PS[2:4])
            NSLOT = 3
            SL = []
            for s_ in range(NSLOT):
                ox_ = sb(f"oxtile{s_}", [128, D], F32, esF)
                SL.append(dict(
                    f32=Ring([sb(f"f32_{s_}_{i}", [128, D], F32, esF) for i in range(3)]),
                    o=ox_, x=ox_,
                    p=sb(f"ptile{s_}", [128, 256], F32, esF), bfT=sb(f"bfT{s_}", [128, 8, 128], BF16, esF),
                    pT=sb(f"pT{s_}", [128, 2, 128], BF16, esF), rt=sb(f"rt{s_}", [128, 96], F32, esF),
                    mlo=sb(f"mlo{s_}", [128, 8, 128], BF16, esF), ps=PS[s_]))
            sqs = sb("sqs", [128, D], BF16, esF)
            sgt_r = Ring([sb(f"sgt{i}", [128, 512], F32, esF) for i in range(2)])
            hid_r = Ring([sb(f"hid{i}", [128, 512], BF16, esF) for i in range(2)])
            ss_r = Ring([sb(f"fss{i}", [128, 1], F32, esF) for i in range(12)])
            rs_r = Ring([sb(f"frs{i}", [128, 1], F32, esF) for i in range(12)])
            wg_r = Ring([sb(f"wg{i}", [128, 8, 256], BF16, esF) for i in range(2)])
            wu_r = Ring([sb(f"wu{i}", [128, 8, 256], BF16, esF) for i in range(2)])
            wd_r = Ring([sb(f"wd{i}", [128, 2, D], BF16, esF) for i in range(2)])

            def rstd_of(src, reads, n):
                ss, rs = ss_r.next(), rs_r.next()
                P.op("act", lambda e: e.activation(out=sqs[:, 0:n], in_=src, func=AF.Square, accum_out=ss[:]),
                     reads=reads, writes=[sqs, ss])
                rstd_from_ss(ss, rs, n)
                return rs

            def transpose8(dst, src, n=8):
                for c in range(n):
                    P.op("pe", lambda e, c=c: e.transpose(dst[:, c * 128:(c + 1) * 128], src[:, c * 128:(c + 1) * 128], idf[:]),
                         reads=[src, idf], writes=[dst], partial=True, signal=(c == n - 1))

            def mm16(dst, lT, w_, nck=8):
                for half in range(2):
                    for c in range(nck):
                        P.op("pe", lambda e, half=half, c=c: e.matmul(
                            dst[:, half * 512:(half + 1) * 512], lhsT=lT[:, c, :], rhs=w_[:, c, half * 512:(half + 1) * 512],
                            start=(c == 0), stop=(c == nck - 1)),
                            reads=[lT, w_], writes=[dst], partial=True, signal=(half == 1 and c == nck - 1))

            def load_expert(e_):
                wg, wu, wd = wg_r.next(), wu_r.next(), wd_r.next()
                P.dma("pool", wg[:], weg_d[e_].rearrange("(c p) n -> p c n", p=128), writes=[wg])
                P.dma("pool", wu[:], weu_d[e_].rearrange("(c p) n -> p c n", p=128), writes=[wu])
                P.dma("pool", wd[:], wed_d[e_].rearrange("(c p) n -> p c n", p=128), writes=[wd])
                return (wg, wu, wd)

            W_next = {}
            n_blk = int(os.environ.get("K_NBLK", NBLK))
            n_exp = int(os.environ.get("K_NEXP", NE))
            def p5_tile(b, tl, sl):
                t = b * TPB + tl
                rows = slice(t * 128, (t + 1) * 128)
                ot_, xt = sl["o"], sl["x"]
                P.dma("sp", ot_[:], o_scr[rows, :], reads=[o_scr_b], writes=[ot_])
                yield
                ra = rstd_of(ot_[:, 0:512], [ot_], 512)
                yield
                rb = rstd_of(ot_[:, 512:1024], [ot_], 512)
                yield
                mx = sl["f32"].next()
                for (r_, lo) in ((ra, 0), (rb, 512)):
                    P.op("dve", lambda e, r_=r_, lo=lo, ot_=ot_, mx=mx: e.scalar_tensor_tensor(
                        out=mx[:, lo:lo + 512], in0=ot_[:, lo:lo + 512], scalar=r_[:, 0:1], in1=gout[:, lo:lo + 512],
                        op0=ALU.mult, op1=ALU.mult), reads=[ot_, r_, gout], writes=[mx], partial=True)
                P.dma("sp", xt[:], x_d[rows, :], writes=[xt])
                yield
                pa = sl["ps"]
                transpose8(pa, mx)
                yield
                mixT = sl["bfT"]
                P.op("act", lambda e, pa=pa, mixT=mixT: e.activation(
                    out=mixT[:], in_=pa[:].rearrange("p (c t) -> p c t", c=8), func=AF.Copy), reads=[pa], writes=[mixT])
                yield
                ph = sl["ps"]
                mm16(ph, mixT, wo)
                yield
                ya = yacc[tl]
                P.op("dve", lambda e, ph=ph, xt=xt, ya=ya: e.tensor_tensor(out=ya[:], in0=ph[:], in1=xt[:], op=ALU.add),
                     reads=[ph, xt], writes=[ya])
                yield
                if dbg and dbg[0] == "h1":
                    _merge(final, P.dma("sp", dbg_d[rows, :], ya[:], reads=[ya]))
                rm = rstd_of(ya[:], [ya], D)
                yield
                mf = sl["f32"].next()
                P.op("dve", lambda e, ya=ya, rm=rm, mf=mf: e.scalar_tensor_tensor(
                    out=mf[:], in0=ya[:], scalar=rm[:, 0:1], in1=gffn[:], op0=ALU.mult, op1=ALU.mult),
                    reads=[ya, rm, gffn], writes=[mf])
                yield
                pm = sl["ps"]
                transpose8(pm, mf)
                yield
                mTf = sl["f32"].next()
                P.op("act", lambda e, pm=pm, mTf=mTf: e.activation(out=mTf[:], in_=pm[:], func=AF.Copy), reads=[pm], writes=[mTf])
                yield
                P.op("dve", lambda e, pm=pm, tl=tl: e.tensor_copy(
                    out=mTb[:, :, tl * 128:(tl + 1) * 128], in_=pm[:].rearrange("p (c t) -> p c t", c=8)),
                    reads=[pm], writes=[mTb], partial=True)
                yield
                mlo = sl["mlo"]
                P.op("dve", lambda e, mTf=mTf, mlo=mlo, tl=tl: e.tensor_tensor(
                    out=mlo[:], in0=mTf[:].rearrange("p (c t) -> p c t", c=8), in1=mTb[:, :, tl * 128:(tl + 1) * 128],
                    op=ALU.subtract), reads=[mTf, mTb], writes=[mlo])
                yield
                pr = sl["ps"]
                k = 0
                for (lt_, w_) in (("hi", wrh), ("lo", wrh), ("hi", wrl)):
                    for c in range(8):
                        k += 1
                        P.op("pe", lambda e, pr=pr, mlo=mlo, c=c, lt_=lt_, w_=w_, tl=tl, k=k: e.matmul(
                            pr[:, 0:36], lhsT=(mTb[:, c, tl * 128:(tl + 1) * 128] if lt_ == "hi" else mlo[:, c, :]),
                            rhs=w_[:, c, :], start=(k == 1), stop=(k == 24)),
                            reads=[mTb, mlo, w_], writes=[pr], partial=True, signal=(k == 24))
                rt = sl["rt"]
                cwb = cw[b]

                def R(eng, fn, extra_r=(), extra_w=()):
                    P.op(eng, fn, reads=list(extra_r), writes=[rt] + list(extra_w), partial=True)
                R("dve", lambda e, pr=pr, rt=rt: e.tensor_tensor(out=rt[:, 0:36], in0=pr[:, 0:36], in1=brb[:], op=ALU.add), [pr, brb])
                yield
                R("dve", lambda e, rt=rt: e.tensor_reduce(out=rt[:, 36:37], in_=rt[:, 0:4], axis=AX.X, op=ALU.max))
                yield
                R("dve", lambda e, rt=rt: e.tensor_scalar(out=rt[:, 37:38], in0=rt[:, 36:37], scalar1=-1.0, scalar2=None, op0=ALU.mult))
                yield
                R("act", lambda e, rt=rt: e.activation(out=rt[:, 88:92], in_=rt[:, 0:4], func=AF.Exp, bias=rt[:, 37:38],
                                                      accum_out=rt[:, 38:39]))
                yield
                R("dve", lambda e, rt=rt: e.reciprocal(out=rt[:, 39:40], in_=rt[:, 38:39]))
                yield
                R("dve", lambda e, rt=rt: e.tensor_scalar(out=rt[:, 40:44], in0=rt[:, 0:4], scalar1=rt[:, 36:37], scalar2=None,
                                                         op0=ALU.is_equal))
                yield
                R("dve", lambda e, rt=rt: e.tensor_scalar(out=rt[:, 44:52], in0=rt[:, 4:12], scalar1=rt[:, 40:41], scalar2=None,
                                                         op0=ALU.mult))
                yield
                for g_ in range(1, 4):
                    R("dve", lambda e, rt=rt, g_=g_: e.scalar_tensor_tensor(
                        out=rt[:, 44:52], in0=rt[:, 4 + 8 * g_:12 + 8 * g_], scalar=rt[:, 40 + g_:41 + g_], in1=rt[:, 44:52],
                        op0=ALU.mult, op1=ALU.add))
                R("dve", lambda e, rt=rt: e.max(out=rt[:, 52:60], in_=rt[:, 44:52]))
                yield
                R("dve", lambda e, rt=rt: e.tensor_scalar(out=rt[:, 60:61], in0=rt[:, 52:53], scalar1=-1.0, scalar2=None, op0=ALU.mult))
                yield
                R("act", lambda e, rt=rt: e.activation(out=rt[:, 64:72], in_=rt[:, 44:52], func=AF.Exp, bias=rt[:, 60:61]))
                yield
                R("dve", lambda e, rt=rt: e.tensor_scalar(out=rt[:, 72:80], in0=rt[:, 44:52], scalar1=rt[:, 53:54], scalar2=None,
                                                         op0=ALU.is_ge))
                yield
                R("dve", lambda e, rt=rt: e.tensor_tensor(out=rt[:, 80:88], in0=rt[:, 64:72], in1=rt[:, 72:80], op=ALU.mult))
                yield
                R("dve", lambda e, rt=rt: e.tensor_reduce(out=rt[:, 61:62], in_=rt[:, 80:88], axis=AX.X, op=ALU.add))
                yield
                R("dve", lambda e, rt=rt: e.reciprocal(out=rt[:, 62:63], in_=rt[:, 61:62]))
                yield
                R("dve", lambda e, rt=rt: e.tensor_tensor(out=rt[:, 63:64], in0=rt[:, 62:63], in1=rt[:, 39:40], op=ALU.mult))
                yield
                R("dve", lambda e, rt=rt: e.tensor_scalar(out=rt[:, 80:88], in0=rt[:, 80:88], scalar1=rt[:, 63:64], scalar2=None,
                                                         op0=ALU.mult))
                yield
                for g_ in range(4):
                    R("dve", lambda e, rt=rt, g_=g_, tl=tl, cwb=cwb: e.tensor_scalar(
                        out=cwb[:, tl, g_ * 8:(g_ + 1) * 8], in0=rt[:, 80:88], scalar1=rt[:, 40 + g_:41 + g_], scalar2=None,
                        op0=ALU.mult), extra_w=[cwb])
                if dbg and dbg[0] == "cw" and b == 0:
                    _merge(final, P.dma("sp", dbg_d[:, tl * 32:(tl + 1) * 32], cwb[:, tl, :], reads=[cwb]))

                yield

            def moe_block(b):
                units = [(e_, sk) for e_ in range(n_exp) for sk in range(NSUB)]
                W = {}
                if units:
                    W[0] = W_next.pop(0) if 0 in W_next else load_expert(0)
                state = {}

                def s1(u):
                    e_, sk = units[u]
                    if sk == 1 and e_ + 1 == n_exp and b + 1 < n_blk:
                        W_next[0] = load_expert(0)
                    if sk == 1 and e_ + 1 < n_exp:
                        W[e_ + 1] = load_expert(e_ + 1)
                    wg, wu, wd = W[e_]
                    gu = gu_r.next()
                    k = 0
                    for gi, w_ in ((0, wg), (1, wu)):
                        for fc in range(2):
                            for c in range(8):
                                k += 1
                                P.op("pe", lambda e, gu=gu, w_=w_, gi=gi, fc=fc, c=c, sk=sk: e.matmul(
                                    gu[:, (gi * 2 + fc) * 256:(gi * 2 + fc + 1) * 256], lhsT=w_[:, c, fc * 128:(fc + 1) * 128],
                                    rhs=mTb[:, c, sk * 256:(sk + 1) * 256], start=(c == 0), stop=(c == 7)),
                                    reads=[w_, mTb], writes=[gu], partial=True, signal=(k == 32))
                    state[u] = gu

                def s2(u):
                    gu = state[u]
                    sgt, hid = sgt_r.next(), hid_r.next()
                    P.op("act", lambda e: e.activation(out=sgt[:], in_=gu[:, 0:512], func=AF.Silu), reads=[gu], writes=[sgt])
                    P.op("dve", lambda e: e.tensor_tensor(out=hid[:], in0=sgt[:], in1=gu[:, 512:1024], op=ALU.mult),
                         reads=[sgt, gu], writes=[hid])
                    state[u] = hid

                def s3(u):
                    e_, sk = units[u]
                    hid = state.pop(u)
                    wd = W[e_][2]
                    for tt in range(2):
                        tl = sk * 2 + tt
                        y = y_r.next()
                        for half in range(2):
                            for fc in range(2):
                                P.op("pe", lambda e, y=y, half=half, fc=fc, tt=tt: e.matmul(
                                    y[:, half * 512:(half + 1) * 512], lhsT=hid[:, fc * 256 + tt * 128:fc * 256 + (tt + 1) * 128],
                                    rhs=wd[:, fc, half * 512:(half + 1) * 512], start=(fc == 0), stop=(fc == 1)),
                                    reads=[hid, wd], writes=[y], partial=True, signal=(half == 1 and fc == 1))
                        ya = yacc[tl]
                        cwb_ = cw[b]
                        P.op("dve", lambda e, y=y, ya=ya, tl=tl, e_=e_, cwb_=cwb_: e.scalar_tensor_tensor(
                            out=ya[:], in0=y[:], scalar=cwb_[:, tl, e_:e_ + 1], in1=ya[:], op0=ALU.mult, op1=ALU.add),
                            reads=[y, cwb_], writes=[ya])

                if units:
                    s1(0)
                for u in range(len(units)):
                    s2(u)
                    if u + 1 < len(units):
                        s1(u + 1)
                    s3(u)


            def p7_tile(b, tl, sl):
                t = b * TPB + tl
                rows = slice(t * 128, (t + 1) * 128)
                h2 = yacc[tl]
                rp = rstd_of(h2[:], [h2], D)
                yield
                mp = sl["f32"].next()
                P.op("dve", lambda e, h2=h2, rp=rp, mp=mp: e.scalar_tensor_tensor(
                    out=mp[:], in0=h2[:], scalar=rp[:, 0:1], in1=gple[:], op0=ALU.mult, op1=ALU.mult),
                    reads=[h2, rp, gple], writes=[mp])
                yield
                pa = sl["ps"]
                transpose8(pa, mp)
                yield
                mpT = sl["bfT"]
                P.op("act", lambda e, pa=pa, mpT=mpT: e.activation(
                    out=mpT[:], in_=pa[:].rearrange("p (c t) -> p c t", c=8), func=AF.Copy), reads=[pa], writes=[mpT])
                yield
                pg = sl["ps"]
                mm16(pg, mpT, wpg)
                yield
                gate = sl["f32"].next()
                P.op("act", lambda e, pg=pg, gate=gate: e.activation(out=gate[:], in_=pg[:], func=AF.Sigmoid), reads=[pg], writes=[gate])
                yield
                pt_ = sl["p"]
                P.dma("sp", pt_[:], p_d[rows, :], writes=[pt_])
                yield
                pp = sl["ps"]
                transpose8(pp, pt_, n=2)
                yield
                pT = sl["pT"]
                P.op("dve", lambda e, pp=pp, pT=pT: e.tensor_copy(
                    out=pT[:], in_=pp[:, 0:256].rearrange("p (c t) -> p c t", c=2)), reads=[pp], writes=[pT])
                yield
                pq = sl["ps"]
                mm16(pq, pT, wpp, nck=2)
                yield
                tmp = sl["f32"].next()
                P.op("dve", lambda e, gate=gate, pq=pq, tmp=tmp: e.tensor_tensor(out=tmp[:], in0=gate[:], in1=pq[:], op=ALU.mult),
                     reads=[gate, pq], writes=[tmp])
                yield
                h3 = sl["f32"].next()
                P.op("dve", lambda e, tmp=tmp, h2=h2, h3=h3: e.tensor_tensor(out=h3[:], in0=tmp[:], in1=h2[:], op=ALU.add),
                     reads=[tmp, h2], writes=[h3])
                yield
                rf = rstd_of(h3[:], [h3], D)
                yield
                ob = sl["f32"].next()
                P.op("dve", lambda e, h3=h3, rf=rf, ob=ob: e.scalar_tensor_tensor(
                    out=ob[:], in0=h3[:], scalar=rf[:, 0:1], in1=gfin[:], op0=ALU.mult, op1=ALU.mult),
                    reads=[h3, rf, gfin], writes=[ob])
                yield
                _merge(final, P.dma("sp", out_d[rows, :], ob[:], reads=[ob]))
                yield
                yield

            def run_window(jobs):
                jobs = list(jobs)
                active = {}
                while jobs or active:
                    for s_ in range(NSLOT):
                        if s_ not in active and jobs:
                            fn, b_, tl_ = jobs.pop(0)
                            active[s_] = fn(b_, tl_, SL[s_])
                    for s_ in list(active):
                        try:
                            next(active[s_])
                        except StopIteration:
                            del active[s_]

            run_window([(p5_tile, 0, tl) for tl in range(TPB)])
            for b in range(n_blk):
                moe_block(b)
                nxt = [(p7_tile, b, tl) for tl in range(TPB)]
                if b + 1 < n_blk:
                    nxt += [(p5_tile, b + 1, tl) for tl in range(TPB)]
                run_window(nxt)
        P.emit(final)
    return nc


def _consts():
    inv_freq = (1.0 / (np.float32(10000.0) ** (np.arange(0, 32, 2, dtype=np.float32) / np.float32(32)))).astype(np.float32)
    ang = (np.arange(S, dtype=np.float32)[:, None] * inv_freq[None, :]).astype(np.float32)
    cos = np.cos(ang).astype(np.float32)
    sin = np.sin(ang).astype(np.float32)
    p = np.arange(128)[:, None]
    f = np.arange(128)[None, :]
    bias = np.zeros((128, 12, 3, 128), np.float32)
    for k in range(12):
        sl = 2.0 ** (3 - k)
        for off in range(3):
            rel = 128 * (off - 1) + p - f
            bias[:, k, off, :] = np.where(np.abs(rel) <= 64, np.exp(-sl * np.abs(rel).astype(np.float64)), 0.0)
    return cos, sin, bias.reshape(128, 12 * 384)


def _in_maps(inputs):
    g = lambda k: np.ascontiguousarray(np.asarray(inputs[k], dtype=np.float32))
    cos, sin, bias = _consts()
    w_r1 = g("w_r1")[0]
    w_r2 = g("w_r2")[0]
    wr = np.ascontiguousarray(np.concatenate([w_r1, w_r2.transpose(1, 0, 2).reshape(D, 32)], axis=1))
    br = np.ascontiguousarray(np.concatenate([g("b_r1")[0], g("b_r2")[0].reshape(32)]))
    tm = lambda a: np.ascontiguousarray(a.reshape(NT, 128, 16).transpose(1, 0, 2).reshape(128, NT * 16))
    tm2 = lambda a: np.ascontiguousarray(np.repeat(a.reshape(NT, 128, 1, 16), 2, axis=2).transpose(1, 0, 2, 3).reshape(128, NT * 32))
    shared = {
        "w_in": g("w_in")[0], "w_uq": g("w_uq")[0], "w_ukv": g("w_ukv")[0], "w_o": g("w_o")[0],
        "wr": wr, "br": br, "w_e_gate": g("w_e_gate")[0], "w_e_up": g("w_e_up")[0],
        "w_e_down": g("w_e_down")[0], "w_ple_gate": g("w_ple_gate")[0], "w_ple_proj": g("w_ple_proj")[0],
        "g_mix": g("g_mix")[0], "g_cq": g("g_cq")[0], "g_ckv": g("g_ckv")[0],
        "g_out": np.ascontiguousarray(np.concatenate([g("g_out_a")[0], g("g_out_b")[0]])),
        "g_ffn": g("g_ffn")[0], "g_ple": g("g_ple")[0], "g_final": g("g_final"),
        "cos_tm": tm(cos), "sin_tm": tm(sin), "cos2_tm": tm2(cos), "sin2_tm": tm2(sin), "bias_a": bias,
    }
    x = g("x")
    p = g("p")[0]
    maps = []
    for b in range(8):
        m = dict(shared)
        m["x"] = x[b]
        m["p"] = p[b]
        maps.append(m)
    return maps


_NC_CACHE = {}


def kernel(**inputs):
    if "nc" not in _NC_CACHE:
        _NC_CACHE["nc"] = build_program()
    nc = _NC_CACHE["nc"]
    res = run_bass_kernel_spmd(nc, _in_maps(inputs), core_ids=list(range(8)))
    return np.stack([np.asarray(r["out"], dtype=np.float32) for r in res.results], axis=0)
```
